# Optimizing a Trainium2 kernel written in Bass

```python
import jax, jax.numpy as jnp
from jax import lax
import numpy as np


D_MODEL = 2048
BATCH = 2
SEQ = 4096
DEPTH = 2

GRID_W = 64
CTX_LEN = 256
HEAD_DIM = 128
N_HEADS = D_MODEL // HEAD_DIM
A_KV_HEADS = 4
A_Q_BLOCK = 128
ROPE_THETA = 10000.0
NA_WIN_ROWS = 8
NA_WIN_COLS = 16
N_EXPERTS = 32
TOP_K = 4
D_EXPERT = D_MODEL
SWIGLU_ALPHA = 1.702
SWIGLU_LIMIT = 7.0
NORM_EPS = 1e-6
N_MIXERS = 2
N_A_LAYERS = (DEPTH + N_MIXERS - 1) // N_MIXERS
N_B_LAYERS = DEPTH // N_MIXERS
N_MOD = 6

kernel_name = 'hybrid_flow_gqa_natten_moe'


def rms_norm(x, g):
    x32 = x.astype(jnp.float32)
    y = x32 * lax.rsqrt(jnp.mean(x32 * x32, axis=-1, keepdims=True) + NORM_EPS)
    return (y * g.astype(jnp.float32)).astype(x.dtype)


def split_heads(t, n_heads):
    b, n_tok, _ = t.shape
    return t.reshape(b, n_tok, n_heads, HEAD_DIM).transpose(0, 2, 1, 3)


def merge_heads(t):
    b, h, n_tok, d = t.shape
    return t.transpose(0, 2, 1, 3).reshape(b, n_tok, h * d)


def rope_1d(x, pos):
    m = x.shape[-1] // 2
    inv_freq = ROPE_THETA ** (-jnp.arange(m, dtype=jnp.float32) / m)
    ang = pos.astype(jnp.float32)[:, None] * inv_freq[None, :]
    cos = jnp.cos(ang).astype(x.dtype)
    sin = jnp.sin(ang).astype(x.dtype)
    x1, x2 = x[..., :m], x[..., m:]
    return jnp.concatenate([x1 * cos - x2 * sin, x2 * cos + x1 * sin], axis=-1)


def axial_rope(x):
    n_tok = x.shape[-2]
    t = jnp.arange(n_tok, dtype=jnp.int32)
    half = x.shape[-1] // 2
    return jnp.concatenate([rope_1d(x[..., :half], t // GRID_W),
                            rope_1d(x[..., half:], t % GRID_W)], axis=-1)


def grouped_softmax_attention(q, k, v):
    s = jnp.einsum('bkgqd,bksd->bkgqs', q, k).astype(jnp.float32) * (HEAD_DIM ** -0.5)
    p = jax.nn.softmax(s, axis=-1).astype(v.dtype)
    return jnp.einsum('bkgqs,bksd->bkgqd', p, v)


def gqa_axial_attention(hx, hc, w_qkv, w_o, q_gain, k_gain, with_ctx):
    B, S, _ = hx.shape
    L = hc.shape[1]
    G = N_HEADS // A_KV_HEADS
    nq = N_HEADS * HEAD_DIM
    nkv = A_KV_HEADS * HEAD_DIM
    qkv_x = hx @ w_qkv
    qx = axial_rope(rms_norm(split_heads(qkv_x[..., :nq], N_HEADS), q_gain))
    kx = axial_rope(rms_norm(split_heads(qkv_x[..., nq:nq + nkv], A_KV_HEADS), k_gain))
    vx = split_heads(qkv_x[..., nq + nkv:], A_KV_HEADS)
    if with_ctx:
        qkv_c = hc @ w_qkv
        qc = rms_norm(split_heads(qkv_c[..., :nq], N_HEADS), q_gain)
        kv_c = qkv_c[..., nq:]
    else:
        kv_c = hc @ w_qkv[:, nq:]
    kc = rms_norm(split_heads(kv_c[..., :nkv], A_KV_HEADS), k_gain)
    vc = split_heads(kv_c[..., nkv:], A_KV_HEADS)
    k_all = jnp.concatenate([kc, kx], axis=2)
    v_all = jnp.concatenate([vc, vx], axis=2)
    nb = S // A_Q_BLOCK
    qb = jnp.moveaxis(qx.reshape(B, A_KV_HEADS, G, nb, A_Q_BLOCK, HEAD_DIM), 3, 0)
    ob = lax.map(lambda q: grouped_softmax_attention(q, k_all, v_all), qb)
    ox = jnp.moveaxis(ob, 0, 3).reshape(B, N_HEADS, S, HEAD_DIM)
    yx = merge_heads(ox) @ w_o
    if with_ctx:
        oc = grouped_softmax_attention(qc.reshape(B, A_KV_HEADS, G, L, HEAD_DIM), kc, vc)
        yc = merge_heads(oc.reshape(B, N_HEADS, L, HEAD_DIM)) @ w_o
        return yx, yc
    return yx, None


def neighborhood_attention(hx, hc, w_qkv, w_o, q_gain, k_gain, rel_bias, with_ctx):
    B, S, _ = hx.shape
    L = hc.shape[1]
    rows = S // GRID_W
    wr = min(NA_WIN_ROWS, rows)
    inner = N_HEADS * HEAD_DIM
    qkv_x = hx @ w_qkv
    qx = rms_norm(split_heads(qkv_x[..., :inner], N_HEADS), q_gain)
    kx = rms_norm(split_heads(qkv_x[..., inner:2 * inner], N_HEADS), k_gain)
    vx = split_heads(qkv_x[..., 2 * inner:], N_HEADS)
    if with_ctx:
        qkv_c = hc @ w_qkv
        qc = rms_norm(split_heads(qkv_c[..., :inner], N_HEADS), q_gain)
        kv_c = qkv_c[..., inner:]
    else:
        kv_c = hc @ w_qkv[:, inner:]
    kc = rms_norm(split_heads(kv_c[..., :inner], N_HEADS), k_gain)
    vc = split_heads(kv_c[..., inner:], N_HEADS)

    kg = kx.reshape(B, N_HEADS, rows, GRID_W, HEAD_DIM)
    vg = vx.reshape(B, N_HEADS, rows, GRID_W, HEAD_DIM)
    q_rows = jnp.moveaxis(qx.reshape(B, N_HEADS, rows, GRID_W, HEAD_DIM), 2, 0)
    col = jnp.arange(GRID_W, dtype=jnp.int32)
    c_start = jnp.clip(col - NA_WIN_COLS // 2, 0, GRID_W - NA_WIN_COLS)
    col_mask = (col[None, :] >= c_start[:, None]) & (col[None, :] < c_start[:, None] + NA_WIN_COLS)
    dc_idx = jnp.clip(col[None, :] - col[:, None], -(NA_WIN_COLS - 1), NA_WIN_COLS - 1) + NA_WIN_COLS - 1
    bias_tab = rel_bias.astype(jnp.float32)
    scale = HEAD_DIM ** -0.5

    def row_block(args):
        q_r, r = args
        r0 = jnp.clip(r - wr // 2, 0, rows - wr)
        kb = lax.dynamic_slice_in_dim(kg, r0, wr, axis=2)
        vb = lax.dynamic_slice_in_dim(vg, r0, wr, axis=2)
        dr_idx = r0 + jnp.arange(wr, dtype=jnp.int32) - r + NA_WIN_ROWS - 1
        bias = jnp.take(jnp.take(bias_tab, dr_idx, axis=1), dc_idx, axis=2)
        bias = bias.transpose(0, 2, 1, 3)
        s_loc = jnp.einsum('bhqd,bhrkd->bhqrk', q_r, kb).astype(jnp.float32) * scale + bias[None]
        s_loc = jnp.where(col_mask[:, None, :], s_loc, -jnp.inf)
        s_ctx = jnp.einsum('bhqd,bhld->bhql', q_r, kc).astype(jnp.float32) * scale
        s = jnp.concatenate([s_loc.reshape(B, N_HEADS, GRID_W, wr * GRID_W), s_ctx], axis=-1)
        p = jax.nn.softmax(s, axis=-1).astype(vb.dtype)
        p_loc = p[..., :wr * GRID_W].reshape(B, N_HEADS, GRID_W, wr, GRID_W)
        p_ctx = p[..., wr * GRID_W:]
        return (jnp.einsum('bhqrk,bhrkd->bhqd', p_loc, vb)
                + jnp.einsum('bhql,bhld->bhqd', p_ctx, vc))

    o_rows = lax.map(row_block, (q_rows, jnp.arange(rows, dtype=jnp.int32)))
    ox = jnp.moveaxis(o_rows, 0, 2).reshape(B, N_HEADS, S, HEAD_DIM)
    yx = merge_heads(ox) @ w_o
    if with_ctx:
        oc = grouped_softmax_attention(qc[:, :, None], kc, vc)[:, :, 0]
        yc = merge_heads(oc) @ w_o
        return yx, yc
    return yx, None


def moe_clamped_swiglu(h, router_w, router_b, w1, b1, w2, b2):
    n_tok = h.shape[0]
    logits = (h @ router_w + router_b).astype(jnp.float32)
    top_v, top_i = lax.top_k(logits, TOP_K)
    top_w = jax.nn.softmax(top_v, axis=-1)
    gates = jnp.zeros((n_tok, N_EXPERTS), jnp.float32).at[
        jnp.arange(n_tok)[:, None], top_i].set(top_w)
    out = jnp.zeros(h.shape, jnp.float32)
    for e in range(N_EXPERTS):
        a = (h @ w1[e] + b1[e]).reshape(n_tok, D_EXPERT, 2)
        glu = jnp.minimum(a[..., 0], SWIGLU_LIMIT)
        lin = jnp.clip(a[..., 1], -SWIGLU_LIMIT, SWIGLU_LIMIT)
        act = glu * jax.nn.sigmoid(SWIGLU_ALPHA * glu) * (lin + 1.0)
        out = out + gates[:, e:e + 1] * (act @ w2[e] + b2[e]).astype(jnp.float32)
    return out.astype(h.dtype)


def modulate(h, shift, scale):
    return h * (1.0 + scale) + shift


def setup_inputs(seed: int = 0) -> dict:
    key = jax.random.key(seed)
    ks = jax.random.split(key, 24)
    D = D_MODEL
    inner = N_HEADS * HEAD_DIM
    a_qkv = (N_HEADS + 2 * A_KV_HEADS) * HEAD_DIM

    def nrm(k, shape, s):
        return jax.random.normal(k, shape, jnp.float32) * s

    return {
        'x': nrm(ks[0], (BATCH, SEQ, D), 1.0),
        'c': nrm(ks[1], (BATCH, D), 1.0),
        'ctx': nrm(ks[2], (BATCH, CTX_LEN, D), 1.0),
        'c_ctx': nrm(ks[3], (D,), 1.0),
        'ada_w': nrm(ks[4], (DEPTH, D, N_MOD * D), 0.5 * D ** -0.5),
        'ada_b': nrm(ks[5], (DEPTH, N_MOD * D), 0.01),
        'norm_mix_g': 1.0 + nrm(ks[6], (DEPTH, D), 0.05),
        'norm_ffn_g': 1.0 + nrm(ks[7], (DEPTH, D), 0.05),
        'a_w_qkv': nrm(ks[8], (N_A_LAYERS, D, a_qkv), D ** -0.5),
        'a_w_o': nrm(ks[9], (N_A_LAYERS, inner, D), inner ** -0.5),
        'a_q_gain': 1.0 + nrm(ks[10], (N_A_LAYERS, HEAD_DIM), 0.05),
        'a_k_gain': 1.0 + nrm(ks[11], (N_A_LAYERS, HEAD_DIM), 0.05),
        'b_w_qkv': nrm(ks[12], (N_B_LAYERS, D, 3 * inner), D ** -0.5),
        'b_w_o': nrm(ks[13], (N_B_LAYERS, inner, D), inner ** -0.5),
        'b_q_gain': 1.0 + nrm(ks[14], (N_B_LAYERS, HEAD_DIM), 0.05),
        'b_k_gain': 1.0 + nrm(ks[15], (N_B_LAYERS, HEAD_DIM), 0.05),
        'b_rel_bias': nrm(ks[16], (N_B_LAYERS, N_HEADS, 2 * NA_WIN_ROWS - 1, 2 * NA_WIN_COLS - 1), 0.1),
        'router_w': nrm(ks[17], (DEPTH, D, N_EXPERTS), D ** -0.5),
        'router_b': nrm(ks[18], (DEPTH, N_EXPERTS), 0.01),
        'exp_w1': nrm(ks[19], (DEPTH, N_EXPERTS, D, 2 * D_EXPERT), D ** -0.5),
        'exp_b1': nrm(ks[20], (DEPTH, N_EXPERTS, 2 * D_EXPERT), 0.01),
        'exp_w2': nrm(ks[21], (DEPTH, N_EXPERTS, D_EXPERT, D), D_EXPERT ** -0.5),
        'exp_b2': nrm(ks[22], (DEPTH, N_EXPERTS, D), 0.01),
    }


def reference(x, c, ctx, c_ctx, ada_w, ada_b, norm_mix_g, norm_ffn_g,
              a_w_qkv, a_w_o, a_q_gain, a_k_gain,
              b_w_qkv, b_w_o, b_q_gain, b_k_gain, b_rel_bias,
              router_w, router_b, exp_w1, exp_b1, exp_w2, exp_b2):
    B, S, D = x.shape
    L = ctx.shape[1]
    for i in range(DEPTH):
        last = i == DEPTH - 1
        mod_x = (jax.nn.silu(c) @ ada_w[i] + ada_b[i])[:, None, :]
        mod_c = (jax.nn.silu(c_ctx) @ ada_w[i] + ada_b[i])[None, None, :]
        sh1x, sc1x, g1x, sh2x, sc2x, g2x = jnp.split(mod_x, N_MOD, axis=-1)
        sh1c, sc1c, g1c, sh2c, sc2c, g2c = jnp.split(mod_c, N_MOD, axis=-1)

        hx = modulate(rms_norm(x, norm_mix_g[i]), sh1x, sc1x)
        hc = modulate(rms_norm(ctx, norm_mix_g[i]), sh1c, sc1c)
        j = i // N_MIXERS
        if i % N_MIXERS == 0:
            yx, yc = gqa_axial_attention(hx, hc, a_w_qkv[j], a_w_o[j], a_q_gain[j], a_k_gain[j],
                                         not last)
        else:
            yx, yc = neighborhood_attention(hx, hc, b_w_qkv[j], b_w_o[j], b_q_gain[j], b_k_gain[j],
                                            b_rel_bias[j], not last)
        x = x + g1x * yx

        hx = modulate(rms_norm(x, norm_ffn_g[i]), sh2x, sc2x)
        if last:
            yx = moe_clamped_swiglu(hx.reshape(B * S, D), router_w[i], router_b[i],
                                    exp_w1[i], exp_b1[i], exp_w2[i], exp_b2[i]).reshape(B, S, D)
        else:
            ctx = ctx + g1c * yc
            hc = modulate(rms_norm(ctx, norm_ffn_g[i]), sh2c, sc2c)
            y_all = moe_clamped_swiglu(
                jnp.concatenate([hx.reshape(B * S, D), hc.reshape(B * L, D)], axis=0),
                router_w[i], router_b[i], exp_w1[i], exp_b1[i], exp_w2[i], exp_b2[i])
            yx = y_all[:B * S].reshape(B, S, D)
            ctx = ctx + g2c * y_all[B * S:].reshape(B, L, D)
        x = x + g2x * yx
    return x
```

```python
from contextlib import ExitStack

import numpy as np
import ml_dtypes
import concourse.bass as bass
import concourse.mybir as mybir
from concourse.bass_utils import run_bass_kernel_spmd

F32 = mybir.dt.float32
BF16 = mybir.dt.bfloat16
AF = mybir.ActivationFunctionType
OP = mybir.AluOpType

D = 2048
NCH = 16
S = 4096
LCTX = 256
NLAT = 1024
NCTX = 64
NE = 32
EPS = 1e-6
ATT_SCALE = 128 ** -0.5
SWIGLU_ALPHA = 1.702
SWIGLU_LIMIT = 7.0
SEM_EPOCH = 12000


_UID = [0]


def UT(nc, name, shape, dt):
    _UID[0] += 1
    return nc.sbuf_tensor(f"{name}_u{_UID[0]}", shape, dt)


class Ctx:
    def __init__(self, nc, n_dma_sems=16):
        self.nc = nc
        self.eng = {"pe": nc.tensor, "act": nc.scalar, "dve": nc.vector,
                    "pool": nc.gpsimd, "sp": nc.sync}
        self.sem = {e: nc.alloc_semaphore(name=f"s_{e}_0") for e in ("pe", "act", "dve", "pool")}
        self.cnt = {e: 0 for e in self.sem}
        self.epoch = {e: 0 for e in self.sem}
        self.dsem = [nc.alloc_semaphore(name=f"d_{i}") for i in range(n_dma_sems)]
        self.dcnt = [0] * n_dma_sems
        self.drr = 0
        self.waited = {e: {} for e in self.eng}
        self.lastw = {}
        self.readers = {}
        self.n_inst = 0

    def _wait(self, e, tok):
        sem, val, key = tok
        if self.waited[e].get(key, 0) >= val:
            return
        self.eng[e].wait_ge(sem, val)
        self.waited[e][key] = val

    def _deps(self, e, R, W):
        for b in R:
            for t in self.lastw.get(b, {}).values():
                if not (e == "pe" and t[2][0] == "pe"):
                    self._wait(e, t)
        for b in W:
            for t in self.lastw.get(b, {}).values():
                if not (e == "pe" and t[2][0] == "pe"):
                    self._wait(e, t)
            for t in self.readers.get(b, {}).values():
                if not (e == "pe" and t[2][0] == "pe"):
                    self._wait(e, t)

    def _record(self, tok, R, W):
        for b in W:
            self.lastw.setdefault(b, {})[tok[2]] = tok
        for b in R:
            self.readers.setdefault(b, {})[tok[2]] = tok

    def op(self, e, fn, R=(), W=()):
        self._deps(e, R, W)
        if self.cnt[e] >= SEM_EPOCH:
            self.epoch[e] += 1
            self.sem[e] = self.nc.alloc_semaphore(name=f"s_{e}_{self.epoch[e]}")
            self.cnt[e] = 0
        inst = fn()
        self.cnt[e] += 1
        inst.then_inc(self.sem[e], 1)
        tok = (self.sem[e], self.cnt[e], (e, self.epoch[e]))
        self._record(tok, R, W)
        self.n_inst += 1
        return tok

    def dma(self, out, in_, R=(), W=(), q="sp", **kw):
        i = self.drr
        self.drr = (self.drr + 1) % len(self.dsem)
        self._deps(q, R, W)
        if self.dcnt[i] > 0:
            self._wait(q, (self.dsem[i], 16 * self.dcnt[i], ("d", i)))
        self.eng[q].dma_start(out=out, in_=in_, **kw).then_inc(self.dsem[i], 16)
        self.dcnt[i] += 1
        tok = (self.dsem[i], 16 * self.dcnt[i], ("d", i))
        self._record(tok, R, W)
        self.n_inst += 1
        return tok

    def dma_custom(self, q, fn, R=(), W=()):
        i = self.drr
        self.drr = (self.drr + 1) % len(self.dsem)
        self._deps(q, R, W)
        if self.dcnt[i] > 0:
            self._wait(q, (self.dsem[i], 16 * self.dcnt[i], ("d", i)))
        fn(self.eng[q]).then_inc(self.dsem[i], 16)
        self.dcnt[i] += 1
        tok = (self.dsem[i], 16 * self.dcnt[i], ("d", i))
        self._record(tok, R, W)
        self.n_inst += 1
        return tok

    def wait_all(self, e):
        for i, s in enumerate(self.dsem):
            if self.dcnt[i]:
                self._wait(e, (s, 16 * self.dcnt[i], ("d", i)))
        for kk, s in self.sem.items():
            if self.cnt[kk]:
                self._wait(e, (s, self.cnt[kk], (kk, self.epoch[kk])))

    def barrier(self):
        for e in self.eng:
            self.wait_all(e)


class Builder:
    def __init__(self, layer, n_experts=NE, debug_stop=None):
        self.layer = layer
        self.n_experts = n_experts
        self.debug_stop = debug_stop
        nc = self.nc = bass.Bass("TRN2", target_bir_lowering=False)
        self.k = Ctx(nc)
        self.T, self.V, self.A, self.P = nc.tensor, nc.vector, nc.scalar, nc.gpsimd
        self.ps = [nc.alloc_psum_tensor(f"ps{i}", [128, 512], F32) for i in range(8)]
        self.din = {}
        self.sfx = ""
        self.NOWN = NLAT + NCTX if layer == 0 else NLAT
        self.groups = [(0, 512, 0), (512, 512, 0)] + ([(1024, 64, 1)] if layer == 0 else [])

    LAYER_INPUTS = ("norm_g", "qk_gain", "ada_w", "ada_b", "w_qkv", "w_o", "router_w", "router_b",
                    "exp_w1", "exp_b1", "exp_w2", "exp_b2")

    def inp(self, name, shape, dt=F32):
        if name in self.LAYER_INPUTS:
            name = name + self.sfx
        if name not in self.din:
            self.din[name] = self.nc.dram_tensor(name, list(shape), dt, kind="ExternalInput")
        return self.din[name].ap()

    def set_layer(self, layer):
        self.layer = layer
        self.NOWN = NLAT + NCTX if layer == 0 else NLAT
        self.groups = [(0, 512, 0), (512, 512, 0)] + ([(1024, 64, 1)] if layer == 0 else [])

    def mm(self, out, lhsT, rhs, start, stop, R, W, **kw):
        return self.k.op("pe", lambda: self.T.matmul(out, lhsT=lhsT, rhs=rhs, start=start, stop=stop, **kw), R, W)

    def tr(self, out, in_, ident, R, W):
        return self.k.op("pe", lambda: self.T.transpose(out, in_, ident), R, W)

    def act(self, out, in_, func, R, W, **kw):
        return self.k.op("act", lambda: self.A.activation(out, in_, func, **kw), R, W)

    def ts(self, e, out, in0, s1, s2, op0, op1, R, W):
        eng = self.V if e == "dve" else self.P
        if op1 is None:
            return self.k.op(e, lambda: eng.tensor_scalar(out, in0, s1, None, op0), R, W)
        return self.k.op(e, lambda: eng.tensor_scalar(out, in0, s1, s2, op0, op1), R, W)

    def tt(self, e, out, in0, in1, op, R, W):
        eng = self.V if e == "dve" else self.P
        return self.k.op(e, lambda: eng.tensor_tensor(out, in0, in1, op), R, W)

    def stt(self, out, in0, scalar, in1, op0, op1, R, W):
        return self.k.op("dve", lambda: self.V.scalar_tensor_tensor(out, in0, scalar, in1, op0, op1), R, W)

    def cp(self, e, out, in_, R, W):
        if e == "act":
            return self.act(out, in_, AF.Copy, R, W)
        eng = self.V if e == "dve" else self.P
        return self.k.op(e, lambda: eng.tensor_copy(out, in_), R, W)

    def recip(self, out, in_, R, W):
        return self.k.op("dve", lambda: self.V.reciprocal(out, in_), R, W)

    def setup_consts(self, es):
        nc, k = self.nc, self.k
        sb = lambda n, s, d: es.enter_context(UT(nc, n, s, d))
        self.ident = sb("ident", [128, 128], F32)
        self.ones_f = sb("ones_f", [128, 128], F32)
        self.ones_b = sb("ones_b", [128, 128], BF16)
        self.rotT = sb("rotT", [128, 128], BF16)
        self.epsc = sb("epsc", [128, 1], F32)
        k.dma(self.ident[:], self.inp("c_ident", [128, 128]), W=["ident"])
        k.dma(self.ones_f[:], self.inp("c_ones", [128, 128]), W=["ones_f"])
        k.dma(self.rotT[:], self.inp("c_rotT", [128, 128], BF16), W=["rotT"])
        k.dma(self.epsc[:], self.inp("c_eps", [128, 1]), W=["epsc"])
        self.cp("dve", self.ones_b[:], self.ones_f[:], ["ones_f"], ["ones_b"])

    def load_cols(self, dst, src2d, n, key, tmp, ps, pskey):
        self.k.dma(tmp[0:n, :], src2d, W=["lc_tmp"])
        self.tr(ps[:, 0:n], tmp[0:n, :], self.ident[0:n, 0:n], ["lc_tmp", "ident"], [pskey])
        self.cp("dve", dst, ps[:, 0:n], [pskey], [key])

    def setup_vectors(self, es, es_tmp):
        nc, k = self.nc, self.k
        sb = lambda n, s, d: es.enter_context(UT(nc, n, s, d))
        sbt = lambda n, s, d: es_tmp.enter_context(UT(nc, n, s, d))
        self.gT = sb("gT", [128, 2, 16], F32)
        self.qkg = sb("qkg", [128, 2], F32)
        self.modT = sb("modT", [128, 96, 2], F32)
        self.modA = sb("modA", [128, 2, 2, 16], F32)
        tmp = sbt("lc_tmp", [128, 128], F32)
        ps = self.ps[7]
        gsrc = self.inp("norm_g", [2, D])
        self.load_cols(self.gT[:].rearrange("p a c -> p (a c)"), gsrc.rearrange("a (c p) -> (a c) p", p=128), 32,
                       "gT", tmp, ps, "ps7")
        self.load_cols(self.qkg[:], self.inp("qk_gain", [2, 128]), 2, "qkg", tmp, ps, "ps7")
        cT = sbt("cT", [128, 2, 16], F32)
        self.load_cols(cT[:].rearrange("p a c -> p (a c)"), self.inp("cvec", [2, D]).rearrange("a (c p) -> (a c) p", p=128),
                       32, "cT", tmp, ps, "ps7")
        scT = sbt("scT", [128, 2, 16], F32)
        self.act(scT[:], cT[:], AF.Silu, ["cT"], ["scT"])
        abT = sbt("abT", [128, 96], F32)
        self.load_cols(abT[:], self.inp("ada_b", [6 * D]).rearrange("(c p) -> c p", p=128), 96, "abT", tmp, ps, "ps7")
        adaw = self.inp("ada_w", [D, 6 * D]).rearrange("(c p) m -> p c m", p=128)
        wst = [sbt(f"adaw{i}", [128, 16, 512], F32) for i in range(2)]
        mps = self.ps[6]
        for blk in range(24):
            w = wst[blk % 2]
            wk = f"adaw{blk % 2}"
            for h in range(2):
                k.dma(w[:, h * 8:(h + 1) * 8, :], adaw[:, h * 8:(h + 1) * 8, blk * 512:(blk + 1) * 512], W=[wk])
            for mc in range(4):
                m = blk * 4 + mc
                for kc in range(16):
                    self.mm(mps[:, 2 * m:2 * m + 2], w[:, kc, mc * 128:(mc + 1) * 128], scT[:, :, kc],
                            kc == 0, kc == 15, [wk, "scT"], ["ps6"])
        for j in range(2):
            self.tt("dve", self.modT[:, :, j], mps[:, 0:192].rearrange("p (m j) -> p m j", j=2)[:, :, j], abT[:],
                    OP.add, ["ps6", "abT"], ["modT"])
        for n in range(2):
            for j in range(2):
                sc = self.modT[:, (3 * n + 1) * 16:(3 * n + 2) * 16, j]
                self.stt(self.modA[:, n, j, :], sc, 1.0, self.gT[:, n, :], OP.add, OP.mult, ["modT", "gT"], ["modA"])

    def mA(self, n, j, c):
        return self.modA[:, n, j, c:c + 1]

    def mB(self, n, j, c):
        return self.modT[:, 3 * n * 16 + c, j:j + 1]

    def mG(self, n, j, c):
        return self.modT[:, (3 * n + 2) * 16 + c, j:j + 1]

    def norm_mod(self, src, skey, ncols, n_idx, j, dst_fn, tmps, ps, pskey, f32_fn=None, post_fn=None):
        sq, rs, tmp = tmps["sq"], tmps["rs"], tmps["tmp"]
        for kc in range(16):
            i = kc % 2
            self.act(sq[i][:, :ncols], src(kc), AF.Square, [skey(kc)], [f"sq{i}"])
            self.mm(ps[:, :ncols], self.ones_f[:], sq[i][:, :ncols], kc == 0, kc == 15, [f"sq{i}", "ones_f"], [pskey])
        self.act(rs[:, :ncols], ps[:, :ncols], AF.Sqrt, [pskey, "epsc"], ["rs"], scale=1.0 / D, bias=self.epsc[:, 0:1])
        self.recip(rs[:, :ncols], rs[:, :ncols], ["rs"], ["rs"])
        for kc in range(16):
            i = kc % 2
            self.tt("dve", tmp[i][:, :ncols], src(kc), rs[:, :ncols], OP.mult, [skey(kc), "rs"], [f"tmp{i}"])
            out, okey = dst_fn(kc)
            if f32_fn is None:
                self.act(out, tmp[i][:, :ncols], AF.Identity, [f"tmp{i}", "modA", "modT"], [okey],
                         scale=self.mA(n_idx, j, kc), bias=self.mB(n_idx, j, kc))
            else:
                h32, hkey = f32_fn(kc)
                self.act(h32, tmp[i][:, :ncols], AF.Identity, [f"tmp{i}", "modA", "modT"], [hkey],
                         scale=self.mA(n_idx, j, kc), bias=self.mB(n_idx, j, kc))
                self.cp("pool", out, h32, [hkey], [okey])
            if post_fn is not None:
                post_fn(kc)

    def qk_finish(self, pps, ppkey, ncols, gain_col, rope, dst, dkey, tm, cos=None, sin=None, cskey=None):
        ps_ss, ps_rot = self.ps[4], self.ps[5]
        self.act(tm["ksq"][:, :ncols], pps, AF.Square, [ppkey], ["ksq"])
        self.mm(ps_ss[:, :ncols], self.ones_f[:], tm["ksq"][:, :ncols], True, True, ["ksq", "ones_f"], ["ps4"])
        self.act(tm["krs"][:, :ncols], ps_ss[:, :ncols], AF.Sqrt, ["ps4", "epsc"], ["krs"], scale=1.0 / 128, bias=self.epsc[:, 0:1])
        self.recip(tm["krs"][:, :ncols], tm["krs"][:, :ncols], ["krs"], ["krs"])
        if not rope:
            self.stt(dst, pps, self.qkg[:, gain_col:gain_col + 1], tm["krs"][:, :ncols], OP.mult, OP.mult,
                     [ppkey, "krs", "qkg"], [dkey])
            return
        self.stt(tm["kn"][:, :ncols], pps, self.qkg[:, gain_col:gain_col + 1], tm["krs"][:, :ncols], OP.mult, OP.mult,
                 [ppkey, "krs", "qkg"], ["kn"])
        self.mm(ps_rot[:, :ncols], self.rotT[:], tm["kn"][:, :ncols], True, True, ["kn", "rotT"], ["ps5"])
        self.tt("pool", tm["t1"][:, :ncols], tm["kn"][:, :ncols], cos, OP.mult, ["kn", cskey], ["t1"])
        self.tt("dve", tm["t2"][:, :ncols], ps_rot[:, :ncols], sin, OP.mult, ["ps5", cskey], ["t2"])
        self.tt("pool", dst, tm["t1"][:, :ncols], tm["t2"][:, :ncols], OP.add, ["t1", "t2"], [dkey])

    def load_w(self, src_ap, stage, skey, dst, dkey, cast_eng="act", deint=False):
        self.k.dma(stage, src_ap, W=[skey])
        if not deint:
            self.cp(cast_eng, dst, stage, [skey], [dkey])
        else:
            sv = stage.rearrange("p c (f two) -> p c two f", two=2)
            self.cp("act", dst[:, :, 0, :], sv[:, :, 0, :], [skey], [dkey])
            self.cp("pool", dst[:, :, 1, :], sv[:, :, 1, :], [skey], [dkey])


def build_layer0(n_experts=NE, debug_stop=None):
    b = Builder(0, n_experts, debug_stop)
    nc, k = b.nc, b.k
    NOWN = b.NOWN
    es_all = ExitStack()
    sbA = lambda n, s, d: es_all.enter_context(UT(nc, n, s, d))
    b.setup_consts(es_all)
    with ExitStack() as es_tmp:
        b.setup_vectors(es_all, es_tmp)
        k.barrier()

    xT = sbA("xT", [128, 16, NOWN], F32)
    xkey = lambda kc, tg: ("xT", kc, tg)
    layer0_body(b, xT, xkey)
    x1 = nc.dram_tensor("x1", [NOWN, D], F32, kind="ExternalOutput").ap()
    store_tokens(b, xT, xkey, x1, NOWN)
    return b


def layer0_body(b, xT, xkey):
    nc, k = b.nc, b.k
    NOWN = b.NOWN
    xall = b.inp("xall", [S, D])
    ctxb = b.inp("ctxb", [LCTX, D])
    cosd = b.inp("cosT", [128, S])
    sind = b.inp("sinT", [128, S])
    wqkv = b.inp("w_qkv", [D, 3072]).rearrange("(c p) m -> p c m", p=128)
    wo = b.inp("w_o", [D, D]).rearrange("(c p) m -> p c m", p=128)
    NKEY = S + LCTX
    kT_d = nc.dram_tensor("kT0_d", [4, 128, NKEY], BF16, kind="Internal").ap()
    v_d = nc.dram_tensor("v0_d", [NKEY, 512], BF16, kind="Internal").ap()

    with ExitStack() as es:
        sb = lambda n, s, d: es.enter_context(UT(nc, n, s, d))
        wkv = sb("wkv", [128, 16, 1024], BF16)
        xs = [sb(f"xs{i}", [128, D], F32) for i in range(2)]
        xTg = [sb(f"xTg{i}", [128, 16, 256], F32) for i in range(2)]
        hxg = [sb(f"hxg{i}", [128, 16, 256], BF16) for i in range(2)]
        tm = {"sq": [sb(f"sq{i}", [128, 256], F32) for i in range(2)], "rs": sb("rs", [128, 256], F32),
              "tmp": [sb(f"tmp{i}", [128, 256], F32) for i in range(2)],
              "ksq": sb("ksq", [128, 256], F32), "krs": sb("krs", [128, 256], F32), "kn": sb("kn", [128, 256], BF16),
              "t1": sb("t1", [128, 256], F32), "t2": sb("t2", [128, 256], F32)}
        cs = [sb(f"cs{i}", [128, 2, 256], F32) for i in range(2)]
        kout = [sb(f"kout{i}", [128, 4, 256], BF16) for i in range(2)]
        vout = [sb(f"vout{i}", [128, 2, 512], BF16) for i in range(2)]
        for q in range(4):
            b.load_w(wqkv[:, :, 2048 + q * 256:2048 + (q + 1) * 256], xTg[q % 2][:], f"xTg{q % 2}",
                     wkv[:, :, q * 256:(q + 1) * 256], "wkv")
        n_groups = 17
        ev = 0
        for g in range(n_groups):
            is_ctx = g == 16
            own = g < 4
            gi = g % 2
            j = 1 if is_ctx else 0
            if not is_ctx:
                k.dma(cs[gi][:, 0, :], cosd[:, g * 256:(g + 1) * 256], W=[f"cs{gi}"])
                k.dma(cs[gi][:, 1, :], sind[:, g * 256:(g + 1) * 256], W=[f"cs{gi}"])
            for t2 in range(2):
                xi = (2 * g + t2) % 2
                rows = ctxb[t2 * 128:(t2 + 1) * 128, :] if is_ctx else xall[g * 256 + t2 * 128:g * 256 + (t2 + 1) * 128, :]
                k.dma(xs[xi][:], rows, W=[f"xs{xi}"])
                for m in range(4):
                    pb = ev % 2
                    for jj in range(4):
                        kc = 4 * m + jj
                        b.tr(b.ps[pb][:, jj * 128:(jj + 1) * 128], xs[xi][:, kc * 128:(kc + 1) * 128], b.ident[:],
                             [f"xs{xi}", "ident"], [f"ps{pb}"])
                    b.cp("act" if ev % 2 == 0 else "dve", xTg[gi][:, 4 * m:4 * m + 4, t2 * 128:(t2 + 1) * 128],
                         b.ps[pb][:].rearrange("p (c n) -> p c n", c=4), [f"ps{pb}"], [f"xTg{gi}"])
                    ev += 1
            if own:
                for h in range(2):
                    b.cp("pool", xT[:, h * 8:(h + 1) * 8, g * 256:(g + 1) * 256], xTg[gi][:, h * 8:(h + 1) * 8, :], [f"xTg{gi}"],
                         [xkey(kc, g // 2) for kc in range(h * 8, (h + 1) * 8)])
            if is_ctx:
                b.cp("pool", xT[:, :, 1024:1088], xTg[gi][:, :, 0:64], [f"xTg{gi}"], [xkey(kc, 2) for kc in range(16)])
            b.norm_mod(lambda kc: xTg[gi][:, kc, :], lambda kc: f"xTg{gi}", 256, 0, j,
                       lambda kc: (hxg[gi][:, kc, :], f"hxg{gi}"), tm, b.ps[2], "ps2")
            for kvh in range(4):
                for kc in range(16):
                    b.mm(b.ps[3][:, :256], wkv[:, kc, kvh * 128:(kvh + 1) * 128], hxg[gi][:, kc, :], kc == 0, kc == 15,
                         ["wkv", f"hxg{gi}"], ["ps3"])
                b.qk_finish(b.ps[3][:, :256], "ps3", 256, 1, not is_ctx, kout[gi][:, kvh, :], f"kout{gi}", tm,
                            cs[gi][:, 0, :], cs[gi][:, 1, :], f"cs{gi}")
            k.dma(kT_d.rearrange("h p t -> p h t")[:, :, g * 256:(g + 1) * 256], kout[gi][:], R=[f"kout{gi}"], W=["kT_d"])
            for t2 in range(2):
                pv = 6 + t2
                for kc in range(16):
                    b.mm(b.ps[pv][:, :], hxg[gi][:, kc, t2 * 128:(t2 + 1) * 128], wkv[:, kc, 512:1024], kc == 0, kc == 15,
                         ["wkv", f"hxg{gi}"], [f"ps{pv}"])
                b.cp("act", vout[gi][:, t2, :], b.ps[pv][:, :], [f"ps{pv}"], [f"vout{gi}"])
            k.dma(v_d[g * 256:(g + 1) * 256, :].rearrange("(t p) c -> p t c", p=128), vout[gi][:], R=[f"vout{gi}"], W=["v_d"])
        k.barrier()

    es_q = ExitStack()
    QT = es_q.enter_context(UT(nc, "QT", [128, 16, NOWN], BF16))
    with ExitStack() as es:
        sb = lambda n, s, d: es.enter_context(UT(nc, n, s, d))
        hx = sb("hx", [128, 16, NOWN], BF16)
        tm = {"sq": [sb(f"sq{i}", [128, 512], F32) for i in range(2)], "rs": sb("rs", [128, 512], F32),
              "tmp": [sb(f"tmp{i}", [128, 512], F32) for i in range(2)],
              "ksq": sb("ksq", [128, 512], F32), "krs": sb("krs", [128, 512], F32), "kn": sb("kn", [128, 512], BF16),
              "t1": sb("t1", [128, 512], F32), "t2": sb("t2", [128, 512], F32)}
        csq = sb("csq", [128, 2, NLAT], F32)
        wst = [sb("wst0", [128, 16, 256], F32)] * 2
        wq = [sb(f"wq{i}", [128, 16, 256], BF16) for i in range(2)]
        k.dma(csq[:, 0, :], cosd[:, 0:NLAT], W=["csq"])
        k.dma(csq[:, 1, :], sind[:, 0:NLAT], W=["csq"])
        for tg, (c0, n, j) in enumerate(b.groups):
            b.norm_mod(lambda kc: xT[:, kc, c0:c0 + n], lambda kc: xkey(kc, tg), n, 0, j,
                       lambda kc: (hx[:, kc, c0:c0 + n], ("hx", tg)), tm, b.ps[2], "ps2")
        for hb in range(8):
            wi = hb % 2
            b.load_w(wqkv[:, :, hb * 256:(hb + 1) * 256], wst[wi][:], "wst0", wq[wi][:], f"wq{wi}")
            for hh in range(2):
                head = 2 * hb + hh
                for tg, (c0, n, j) in enumerate(b.groups):
                    for kc in range(16):
                        b.mm(b.ps[3][:, :n], wq[wi][:, kc, hh * 128:(hh + 1) * 128], hx[:, kc, c0:c0 + n], kc == 0, kc == 15,
                             [f"wq{wi}", ("hx", tg)], ["ps3"])
                    rope = j == 0
                    b.qk_finish(b.ps[3][:, :n], "ps3", n, 0, rope, QT[:, head, c0:c0 + n], ("QT", head, tg), tm,
                                csq[:, 0, c0:c0 + n] if rope else None, csq[:, 1, c0:c0 + n] if rope else None, "csq")
        k.barrier()

    with ExitStack() as es:
        sb = lambda n, s, d: es.enter_context(UT(nc, n, s, d))
        NKT = NKEY // 128
        kts = [sb(f"kts{i}", [128, NKEY], BF16) for i in range(2)]
        vs = [sb(f"vs{i}", [128, NKT, 128], BF16) for i in range(2)]
        pT = [sb(f"pT{i}", [128, 512], BF16) for i in range(3)]
        rinv = sb("rinv", [128, 512], F32)
        it = 0
        for kvh in range(4):
            ki = kvh % 2
            k.dma(kts[ki][:], kT_d[kvh], R=["kT_d"], W=[f"kts{ki}"])
            k.dma(vs[ki][:], v_d.rearrange("(t p) c -> p t c", p=128)[:, :, kvh * 128:(kvh + 1) * 128], R=["v_d"], W=[f"vs{ki}"])
            for h in range(4):
                head = kvh * 4 + h
                for tg, (c0, n, j) in enumerate(b.groups):
                    tiles = list(range(NKT)) if j == 0 else [32, 33]
                    pO, pS = b.ps[4 + it % 2], b.ps[6 + it % 2]
                    kO, kS = f"ps{4 + it % 2}", f"ps{6 + it % 2}"
                    it += 1
                    q_ap = QT[:, head, c0:c0 + n]
                    qkey = ("QT", head, tg)

                    def s_mm(idx):
                        kt = tiles[idx]
                        b.mm(b.ps[idx % 2][:, :n], kts[ki][:, kt * 128:(kt + 1) * 128], q_ap, True, True,
                             [f"kts{ki}", qkey], [f"ps{idx % 2}"])
                    s_mm(0)
                    for idx, kt in enumerate(tiles):
                        if idx + 1 < len(tiles):
                            s_mm(idx + 1)
                        pi = idx % 3
                        b.act(pT[pi][:, :n], b.ps[idx % 2][:, :n], AF.Exp, [f"ps{idx % 2}"], [f"pT{pi}"], scale=ATT_SCALE)
                        b.mm(pO[:, :n], vs[ki][:, kt, :], pT[pi][:, :n], idx == 0, idx == len(tiles) - 1, [f"vs{ki}", f"pT{pi}"], [kO])
                        b.mm(pS[:, :n], b.ones_b[:], pT[pi][:, :n], idx == 0, idx == len(tiles) - 1, ["ones_b", f"pT{pi}"], [kS])
                    b.recip(rinv[:, :n], pS[:, :n], [kS], ["rinv"])
                    b.tt("dve", q_ap, pO[:, :n], rinv[:, :n], OP.mult, [kO, "rinv"], [qkey])
        k.barrier()

    with ExitStack() as es:
        sb = lambda n, s, d: es.enter_context(UT(nc, n, s, d))
        wst = [sb(f"wst{i}", [128, 16, 256], F32) for i in range(2)]
        wob = [sb(f"wob{i}", [128, 16, 256], BF16) for i in range(2)]
        it = 0
        for db in range(8):
            wi = db % 2
            b.load_w(wo[:, :, db * 256:(db + 1) * 256], wst[wi][:], f"wst{wi}", wob[wi][:], f"wob{wi}")
            for dcl in range(2):
                dc = 2 * db + dcl
                for tg, (c0, n, j) in enumerate(b.groups):
                    pb = it % 2
                    it += 1
                    for hc in range(16):
                        b.mm(b.ps[pb][:, :n], wob[wi][:, hc, dcl * 128:(dcl + 1) * 128], QT[:, hc, c0:c0 + n], hc == 0, hc == 15,
                             [f"wob{wi}", ("QT", hc, tg)], [f"ps{pb}"])
                    b.stt(xT[:, dc, c0:c0 + n], b.ps[pb][:, :n], b.mG(0, j, dc), xT[:, dc, c0:c0 + n], OP.mult, OP.add,
                          [f"ps{pb}", "modT", xkey(dc, tg)], [xkey(dc, tg)])
        k.barrier()

    es_q.close()
    if b.debug_stop != "mix":
        moe(b, xT, xkey, None)


NHALO_T, NHALO_B = 256, 192
NK1 = NLAT + NHALO_T + NHALO_B + LCTX
NEG = -30000.0


def _kcol(lr):
    if lr < 4:
        return NLAT + lr * 64
    if lr < 20:
        return (lr - 4) * 64
    return NLAT + NHALO_T + (lr - 20) * 64


def _lrs(j):
    if j < 4:
        return list(range(j, 12))
    if j <= 12:
        return list(range(j, j + 8))
    return list(range(12, j + 8))


def build_layer1(n_experts=NE, debug_stop=None):
    b = Builder(1, n_experts, debug_stop)
    nc, k = b.nc, b.k
    es_all = ExitStack()
    sbA = lambda n, s, d: es_all.enter_context(UT(nc, n, s, d))
    b.setup_consts(es_all)
    rvb = sbA("rvb", [128, 16, 12], F32)
    k.dma(rvb[:], b.inp("rowbias", [128, 16, 12]), W=["rvb"])
    with ExitStack() as es_tmp:
        b.setup_vectors(es_all, es_tmp)
        k.barrier()
    xT = sbA("xT", [128, 16, NLAT], F32)
    xkey = lambda kc, tg: ("xT", kc, tg)
    layer1_body(b, xT, xkey, rvb)
    yout = nc.dram_tensor("yout", [NLAT, D], F32, kind="ExternalOutput").ap()
    store_tokens(b, xT, xkey, yout, NLAT)
    return b


def layer1_body(b, xT, xkey, rvb, gather=None):
    nc, k = b.nc, b.k
    if gather is None:
        xown = b.inp("xown", [NLAT, D])
        xoth = b.inp("xoth", [NHALO_T + NHALO_B + LCTX, D])
    wqkv = b.inp("w_qkv", [D, 3 * D]).rearrange("(c p) m -> p c m", p=128)
    wo = b.inp("w_o", [D, D]).rearrange("(c p) m -> p c m", p=128)
    tbl_d = b.inp("bias_tbl", [16, 64, 15, 64])
    kT_d = nc.dram_tensor("kT1_d", [16, 128, NK1], BF16, kind="Internal").ap()
    v_d = nc.dram_tensor("v1_d", [NK1, D], BF16, kind="Internal").ap()
    es_q = ExitStack()
    QT = es_q.enter_context(UT(nc, "QT", [128, 16, NLAT], BF16))

    with ExitStack() as es:
        sb = lambda n, s, d: es.enter_context(UT(nc, n, s, d))
        hx = sb("hx", [128, 16, NLAT], BF16)
        xs = [sb(f"xs{i}", [128, D], F32) for i in range(2)]
        wst = sb("wst0", [128, 16, 256], F32)
        xTg = wst
        wb = [sb(f"wb{i}", [128, 16, 256], BF16) for i in range(2)]
        tm = {"sq": [sb(f"sq{i}", [128, 512], F32) for i in range(2)], "rs": sb("rs", [128, 512], F32),
              "tmp": [sb(f"tmp{i}", [128, 512], F32) for i in range(2)],
              "ksq": sb("ksq", [128, 512], F32), "krs": sb("krs", [128, 512], F32)}
        kout = [sb(f"kout{i}", [128, 512], BF16) for i in range(2)]
        vout = [sb(f"vout{i}", [128, 256], BF16) for i in range(2)]
        ev = [0]

        def load_T(src_rows, n, dst_fn, dkeys):
            xi = (ev[0] // 4) % 2
            if isinstance(src_rows, int):
                gath_d, gidx = gather
                k.dma_custom("pool", lambda eng: eng.indirect_dma_start(
                    out=xs[xi][0:n, :], out_offset=None, in_=gath_d[:, :],
                    in_offset=bass.IndirectOffsetOnAxis(ap=gidx[0:n, src_rows:src_rows + 1], axis=0)),
                    R=["gath_d", "gidx"], W=[f"xs{xi}"])
            else:
                k.dma(xs[xi][0:n, :], src_rows, W=[f"xs{xi}"])
            for m in range(4):
                pb = ev[0] % 2
                ev[0] += 1
                for jj in range(4):
                    kc = 4 * m + jj
                    b.tr(b.ps[pb][:, jj * 128:jj * 128 + n], xs[xi][0:n, kc * 128:(kc + 1) * 128], b.ident[0:n, 0:n],
                         [f"xs{xi}", "ident"], [f"ps{pb}"])
                b.cp("act" if m % 2 == 0 else "dve", dst_fn(m), b.ps[pb][:].rearrange("p (c n) -> p c n", c=4)[:, :, 0:n],
                     [f"ps{pb}"], dkeys(m))

        def qkv_pass(pass_id, ngroups):
            koff = pass_id * NLAT
            sections = ([(0, 0)] if pass_id == 0 else []) + [(1, D), (2, 2 * D)]
            wctr = 0
            oc = 0
            for kind, woff in sections:
                for blk in range(8):
                    wi = wctr % 2
                    wctr += 1
                    b.load_w(wqkv[:, :, woff + blk * 256:woff + (blk + 1) * 256], wst[:], "wst0", wb[wi][:], f"wb{wi}")
                    if kind < 2:
                        for hh in range(2):
                            head = 2 * blk + hh
                            for (c0, n) in ngroups:
                                for kc in range(16):
                                    b.mm(b.ps[3][:, :n], wb[wi][:, kc, hh * 128:(hh + 1) * 128], hx[:, kc, c0:c0 + n], kc == 0, kc == 15,
                                         [f"wb{wi}", "hx"], ["ps3"])
                                if kind == 0:
                                    b.qk_finish(b.ps[3][:, :n], "ps3", n, 0, False, QT[:, head, c0:c0 + n], ("QT", head), tm)
                                else:
                                    oi = oc % 2
                                    oc += 1
                                    b.qk_finish(b.ps[3][:, :n], "ps3", n, 1, False, kout[oi][:, :n], f"kout{oi}", tm)
                                    k.dma(kT_d[head][:, koff + c0:koff + c0 + n], kout[oi][:, :n], R=[f"kout{oi}"], W=["kT_d"])
                    else:
                        for (c0, n) in ngroups:
                            for t0 in range(0, n, 128):
                                nt = min(128, n - t0)
                                oi = oc % 2
                                oc += 1
                                pv = 6 + oi
                                for kc in range(16):
                                    b.mm(b.ps[pv][0:nt, 0:256], hx[:, kc, c0 + t0:c0 + t0 + nt], wb[wi][:, kc, :], kc == 0, kc == 15,
                                         [f"wb{wi}", "hx"], [f"ps{pv}"])
                                b.cp("act", vout[oi][0:nt, :], b.ps[pv][0:nt, 0:256], [f"ps{pv}"], [f"vout{oi}"])
                                k.dma(v_d[koff + c0 + t0:koff + c0 + t0 + nt, blk * 256:(blk + 1) * 256], vout[oi][0:nt, :],
                                      R=[f"vout{oi}"], W=["v_d"])

        for t in range(8 if gather is None else 0):
            load_T(xown[t * 128:(t + 1) * 128, :], 128, lambda m, t=t: xT[:, 4 * m:4 * m + 4, t * 128:(t + 1) * 128],
                   lambda m, t=t: [xkey(kc, t // 4) for kc in range(4 * m, 4 * m + 4)])
        for tg, (c0, n, j) in enumerate(b.groups):
            b.norm_mod(lambda kc: xT[:, kc, c0:c0 + n], lambda kc: xkey(kc, tg), n, 0, 0,
                       lambda kc: (hx[:, kc, c0:c0 + n], "hx"), tm, b.ps[2], "ps2")
        qkv_pass(0, [(0, 512), (512, 512)])
        gt = 0
        for (g0, n, j) in ((0, 256, 0), (256, 192, 0), (448, 256, 1)):
            for t0 in range(0, n, 128):
                nt = min(128, n - t0)
                src = xoth[g0 + t0:g0 + t0 + nt, :] if gather is None else gt
                gt += 1
                load_T(src, nt, lambda m, t0=t0, nt=nt: xTg[:, 4 * m:4 * m + 4, t0:t0 + nt], lambda m: ["wst0"])
            b.norm_mod(lambda kc: xTg[:, kc, 0:n], lambda kc: "wst0", n, 0, j,
                       lambda kc: (hx[:, kc, g0:g0 + n], "hx"), tm, b.ps[2], "ps2")
        qkv_pass(1, [(0, 512), (512, 192)])
        k.barrier()

    with ExitStack() as es:
        sb = lambda n, s, d: es.enter_context(UT(nc, n, s, d))
        kts = [sb(f"kts{i}", [128, NK1], BF16) for i in range(2)]
        vs = [sb(f"vs{i}", [64, 23, 128], BF16) for i in range(2)]
        vsc = [sb(f"vsc{i}", [128, 2, 128], BF16) for i in range(2)]
        tbl = [sb(f"tbl{i}", [64, 15, 64], F32) for i in range(2)]
        sbuf = [sb(f"sbf{i}", [64, 12, 64], F32) for i in range(2)]
        pl = [sb(f"pl{i}", [64, 12, 64], BF16) for i in range(2)]
        pc = [sb(f"pc{i}", [128, 2, 64], BF16) for i in range(2)]
        rinv = sb("rinv", [128, 64], F32)
        it = 0
        vloc = v_d[0:NLAT + NHALO_T + NHALO_B, :].rearrange("(r p) c -> p r c", p=64)
        vctx = v_d[NLAT + NHALO_T + NHALO_B:NK1, :].rearrange("(t p) c -> p t c", p=128)
        for h in range(16):
            hi = h % 2
            k.dma(kts[hi][:], kT_d[h], R=["kT_d"], W=[f"kts{hi}"])
            k.dma(vs[hi][:], vloc[:, :, h * 128:(h + 1) * 128], R=["v_d"], W=[f"vs{hi}"])
            k.dma(vsc[hi][:], vctx[:, :, h * 128:(h + 1) * 128], R=["v_d"], W=[f"vsc{hi}"])
            k.dma(tbl[hi][:], tbl_d[h], W=[f"tbl{hi}"])
            for j in range(16):
                ii = it % 2
                it += 1
                lrs = _lrs(j)
                nr = len(lrs)
                edge = j < 4 or j > 12
                q_ap = QT[:, h, j * 64:(j + 1) * 64]
                qkey = ("QT", h)
                pA, pB = b.ps[2 * ii], b.ps[2 * ii + 1]
                kA, kB = f"ps{2 * ii}", f"ps{2 * ii + 1}"
                pO, pS = b.ps[4 + ii], b.ps[6 + ii]
                kO, kS = f"ps{4 + ii}", f"ps{6 + ii}"
                for idx, lr in enumerate(lrs):
                    pp, kk, o = (pA, kA, idx) if idx < 8 else (pB, kB, idx - 8)
                    b.mm(pp[0:64, o * 64:(o + 1) * 64], kts[hi][:, _kcol(lr):_kcol(lr) + 64], q_ap, True, True, [f"kts{hi}", qkey], [kk])
                for c in range(2):
                    c0 = NLAT + NHALO_T + NHALO_B + c * 128
                    b.mm(pB[:, 256 + c * 64:256 + (c + 1) * 64], kts[hi][:, c0:c0 + 128], q_ap, True, True, [f"kts{hi}", qkey], [kB])
                a = lrs[0] - j + 3
                n1 = min(nr, 8)
                b.stt(sbuf[ii][:, 0:n1, :], pA[0:64, 0:n1 * 64].rearrange("p (r q) -> p r q", q=64), ATT_SCALE, tbl[hi][:, a:a + n1, :],
                      OP.mult, OP.add, [kA, f"tbl{hi}"], [f"sbf{ii}"])
                if nr > 8:
                    b.stt(sbuf[ii][:, 8:nr, :], pB[0:64, 0:(nr - 8) * 64].rearrange("p (r q) -> p r q", q=64), ATT_SCALE,
                          tbl[hi][:, a + 8:a + nr, :], OP.mult, OP.add, [kB, f"tbl{hi}"], [f"sbf{ii}"])
                if not edge:
                    b.act(pl[ii][:, 0:nr, :], sbuf[ii][:, 0:nr, :], AF.Exp, [f"sbf{ii}"], [f"pl{ii}"])
                else:
                    for idx in range(nr):
                        b.act(pl[ii][:, idx, :], sbuf[ii][:, idx, :], AF.Exp, [f"sbf{ii}", "rvb"], [f"pl{ii}"], bias=rvb[0:64, j, idx:idx + 1])
                b.act(pc[ii][:], pB[:, 256:384].rearrange("p (c q) -> p c q", q=64), AF.Exp, [kB], [f"pc{ii}"], scale=ATT_SCALE)
                nmm = nr + 2
                for idx, lr in enumerate(lrs):
                    vrow = _kcol(lr) // 64
                    b.mm(pO[:, 0:64], vs[hi][:, vrow, :], pl[ii][:, idx, :], idx == 0, False, [f"vs{hi}", f"pl{ii}"], [kO])
                    b.mm(pS[:, 0:64], b.ones_b[0:64, :], pl[ii][:, idx, :], idx == 0, False, ["ones_b", f"pl{ii}"], [kS])
                for c in range(2):
                    b.mm(pO[:, 0:64], vsc[hi][:, c, :], pc[ii][:, c, :], False, c == 1, [f"vsc{hi}", f"pc{ii}"], [kO])
                    b.mm(pS[:, 0:64], b.ones_b[:], pc[ii][:, c, :], False, c == 1, ["ones_b", f"pc{ii}"], [kS])
                b.recip(rinv[:], pS[:, 0:64], [kS], ["rinv"])
                b.tt("dve", q_ap, pO[:, 0:64], rinv[:], OP.mult, [kO, "rinv"], [qkey])
        k.barrier()

    with ExitStack() as es:
        sb = lambda n, s, d: es.enter_context(UT(nc, n, s, d))
        wst = [sb(f"wst{i}", [128, 16, 256], F32) for i in range(2)]
        wob = [sb(f"wob{i}", [128, 16, 256], BF16) for i in range(2)]
        it = 0
        for db in range(8):
            wi = db % 2
            b.load_w(wo[:, :, db * 256:(db + 1) * 256], wst[wi][:], f"wst{wi}", wob[wi][:], f"wob{wi}")
            for dcl in range(2):
                dc = 2 * db + dcl
                for tg, (c0, n, j) in enumerate(b.groups):
                    pb = it % 2
                    it += 1
                    for hc in range(16):
                        b.mm(b.ps[pb][:, :n], wob[wi][:, hc, dcl * 128:(dcl + 1) * 128], QT[:, hc, c0:c0 + n], hc == 0, hc == 15,
                             [f"wob{wi}", ("QT", hc)], [f"ps{pb}"])
                    b.stt(xT[:, dc, c0:c0 + n], b.ps[pb][:, :n], b.mG(0, j, dc), xT[:, dc, c0:c0 + n], OP.mult, OP.add,
                          [f"ps{pb}", "modT", xkey(dc, tg)], [xkey(dc, tg)])
        k.barrier()
    es_q.close()
    if b.debug_stop != "mix":
        moe(b, xT, xkey, None)


NEDGE = 512
EDGE_TILES = ((0, 128), (128, 64), (768, 128), (896, 128), (1024, 64))


def build_fused(n_experts=NE, debug_stop=None):
    b = Builder(0, n_experts, debug_stop)
    nc, k = b.nc, b.k
    b.sfx = "_l0"
    es_all = ExitStack()
    sbA = lambda n, s, d: es_all.enter_context(UT(nc, n, s, d))
    b.setup_consts(es_all)
    rvb = sbA("rvb", [128, 16, 12], F32)
    gidx = sbA("gidx", [128, 6], mybir.dt.int32)
    k.dma(rvb[:], b.inp("rowbias", [128, 16, 12]), W=["rvb"])
    k.dma(gidx[:], b.inp("gidx", [128, 6], mybir.dt.int32), W=["gidx"])
    with ExitStack() as es_tmp:
        b.setup_vectors(es_all, es_tmp)
        k.barrier()
    xT = sbA("xT", [128, 16, NLAT + NCTX], F32)
    xkey = lambda kc, tg: ("xT", kc, tg)
    layer0_body(b, xT, xkey)

    edge_d = nc.dram_tensor("edge_d", [NEDGE, D], F32, kind="Internal").ap()
    gath_d = nc.dram_tensor("gath_d", [8 * NEDGE, D], F32, kind="Internal").ap()
    with ExitStack() as es:
        ost = [es.enter_context(UT(nc, f"ost{i}", [128, D], F32)) for i in range(2)]
        ev = 0
        row = 0
        for ti, (t0, n) in enumerate(EDGE_TILES):
            tg = min(t0 // 512, 2)
            oi = ti % 2
            for m in range(4):
                pb = ev % 2
                for jj in range(4):
                    kc = 4 * m + jj
                    b.tr(b.ps[pb][0:n, jj * 128:(jj + 1) * 128], xT[:, kc, t0:t0 + n], b.ident[:], [xkey(kc, tg), "ident"], [f"ps{pb}"])
                b.cp("act" if ev % 2 == 0 else "dve", ost[oi][0:n, m * 512:(m + 1) * 512], b.ps[pb][0:n, :], [f"ps{pb}"], [f"ost{oi}"])
                ev += 1
            k.dma(edge_d[row:row + n, :], ost[oi][0:n, :], R=[f"ost{oi}"], W=["edge_d"])
            row += n
        k.barrier()
    k.dma_custom("pool", lambda eng: eng.collective_compute(
        "AllGather", OP.bypass, replica_groups=[list(range(8))], ins=[edge_d[:, :]], outs=[gath_d[:, :]]),
        R=["edge_d"], W=["gath_d"])
    k.barrier()

    b.set_layer(1)
    b.sfx = "_l1"
    with ExitStack() as es_tmp:
        b.setup_vectors(es_all, es_tmp)
        k.barrier()
    layer1_body(b, xT, xkey, rvb, gather=(gath_d, gidx))
    yout = nc.dram_tensor("yout", [NLAT, D], F32, kind="ExternalOutput").ap()
    store_tokens(b, xT, xkey, yout, NLAT)
    return b


def store_tokens(b, xT, xkey, out_d, ntok):
    nc, k = b.nc, b.k
    with ExitStack() as es:
        ost = [es.enter_context(UT(nc, f"ost{i}", [128, D], F32)) for i in range(2)]
        ev = 0
        nt = (ntok + 127) // 128
        for t in range(nt):
            n = min(128, ntok - t * 128)
            tg = min(t // 4, 2)
            oi = t % 2
            for m in range(4):
                pb = ev % 2
                for jj in range(4):
                    kc = 4 * m + jj
                    b.tr(b.ps[pb][0:n, jj * 128:(jj + 1) * 128], xT[:, kc, t * 128:t * 128 + n], b.ident[:],
                         [xkey(kc, tg), "ident"], [f"ps{pb}"])
                b.cp("act" if ev % 2 == 0 else "dve", ost[oi][0:n, m * 512:(m + 1) * 512], b.ps[pb][0:n, :], [f"ps{pb}"], [f"ost{oi}"])
                ev += 1
            k.dma(out_d[t * 128:t * 128 + n, :], ost[oi][0:n, :], R=[f"ost{oi}"], W=["out_d"])
    k.barrier()


def moe(b, xT, xkey, es_all):
    nc, k = b.nc, b.k
    NOWN = b.NOWN
    NT = (NOWN + 127) // 128
    rw_d = b.inp("router_w", [D, NE]).rearrange("(c p) e -> p c e", p=128)
    rb_d = b.inp("router_b", [1, NE])
    w1_d = b.inp("exp_w1", [b.n_experts, D, 2 * D])
    b1_d = b.inp("exp_b1", [NE, 2 * D])
    w2_d = b.inp("exp_w2", [b.n_experts, D, D])
    b2_d = b.inp("exp_b2", [NE, D])
    with ExitStack() as es:
        sb = lambda n, s, d: es.enter_context(UT(nc, n, s, d))
        hx = sb("hx", [128, 16, NOWN], BF16)
        gatesT = sb("gatesT", [NE, NOWN], F32)
        gsel = sb("gsel", [NE, NOWN], F32)
        b1g = sb("b1g", [128, NE * 16], F32)
        b1l = sb("b1l", [128, NE * 16], F32)
        with ExitStack() as es2:
            sb2 = lambda n, s, d: es2.enter_context(UT(nc, n, s, d))
            rw = sb2("rw", [128, 16, NE], F32)
            rb = sb2("rb", [1, NE], F32)
            h32 = [sb2(f"h32_{i}", [128, 512], F32) for i in range(2)]
            tm = {"sq": [sb2(f"sq{i}", [128, 512], F32) for i in range(2)], "rs": sb2("rs", [128, 512], F32),
                  "tmp": [sb2(f"tmp{i}", [128, 512], F32) for i in range(2)]}
            lg = sb2("lg", [128, NT, NE], F32)
            ex = sb2("ex", [128, NT, NE], F32)
            msk = sb2("msk", [128, NT, NE], F32)
            gts = sb2("gts", [128, NT, NE], F32)
            m8 = sb2("m8", [128, NT, 8], F32)
            nmx = sb2("nmx", [128, NT], F32)
            den = sb2("den", [128, NT], F32)
            b1t = sb2("b1t", [128, 256], F32)
            k.dma(rw[:], rw_d, W=["rw"])
            k.dma(rb[:], rb_d, W=["rb"])
            b1v = b1_d.rearrange("e (fc x) -> (e fc) x", x=256)
            for r in range(4):
                k.dma(b1t[:], b1v[r * 128:(r + 1) * 128, :], W=["b1t"])
                bv = b1t[:].rearrange("r (f two) -> r two f", two=2)
                for two, dst in ((0, b1g), (1, b1l)):
                    b.tr(b.ps[7][:, 0:128], bv[:, two, :], b.ident[:], ["b1t", "ident"], ["ps7"])
                    b.cp("dve", dst[:, r * 128:(r + 1) * 128], b.ps[7][:, 0:128], ["ps7"], ["b1"])
            lgp = b.ps[6][:, 0:NT * NE].rearrange("p (t e) -> p t e", e=NE)
            first = [True]
            n_mm = sum(16 * ((n + 127) // 128) for (_, n, _) in b.groups) + NT
            cnt_mm = [0]

            def rmm(out, lhsT, rhs, R):
                cnt_mm[0] += 1
                b.mm(out, lhsT, rhs, first[0], cnt_mm[0] == n_mm, R, ["ps6"], skip_group_check=True)
                first[0] = False

            for tg, (c0, n, j) in enumerate(b.groups):
                def post(kc, c0=c0, n=n):
                    i = kc % 2
                    for t0 in range(0, n, 128):
                        nt = min(128, n - t0)
                        tile = (c0 + t0) // 128
                        rmm(lgp[0:nt, tile, :], h32[i][:, t0:t0 + nt], rw[:, kc, :], [f"h32_{i}", "rw"])
                b.norm_mod(lambda kc: xT[:, kc, c0:c0 + n], lambda kc: xkey(kc, tg), n, 1, j,
                           lambda kc: (hx[:, kc, c0:c0 + n], ("hx", tg)), tm, b.ps[2], "ps2",
                           f32_fn=lambda kc: (h32[kc % 2][:, :n], f"h32_{kc % 2}"), post_fn=post)
            for t in range(NT):
                nt = min(128, NOWN - t * 128)
                rmm(lgp[0:nt, t, :], b.ones_f[0:1, 0:nt], rb[0:1, :], ["ones_f", "rb"])
            b.cp("dve", lg[:], lgp, ["ps6"], ["lg"])
            for t in range(NT):
                nt = min(128, NOWN - t * 128)
                k.op("dve", lambda: b.V.max(out=m8[0:nt, t, :], in_=lg[0:nt, t, :]), ["lg"], ["m8"])
                b.ts("dve", nmx[0:nt, t:t + 1], m8[0:nt, t, 0:1], -1.0, None, OP.mult, None, ["m8"], ["nmx"])
                b.ts("dve", msk[0:nt, t, :], lg[0:nt, t, :], m8[0:nt, t, 3:4], None, OP.is_ge, None, ["lg", "m8"], ["msk"])
                b.act(ex[0:nt, t, :], lg[0:nt, t, :], AF.Exp, ["lg", "nmx"], ["ex"], bias=nmx[0:nt, t:t + 1])
                k.op("dve", lambda: b.V.scalar_tensor_tensor(ex[0:nt, t, :], ex[0:nt, t, :], 1.0, msk[0:nt, t, :], OP.mult, OP.mult,
                                                             accum_out=den[0:nt, t:t + 1]), ["ex", "msk"], ["ex", "den"])
                b.recip(den[0:nt, t:t + 1], den[0:nt, t:t + 1], ["den"], ["den"])
                b.ts("dve", gts[0:nt, t, :], ex[0:nt, t, :], den[0:nt, t:t + 1], None, OP.mult, None, ["ex", "den"], ["gts"])
                b.tr(b.ps[7][0:NE, 0:nt], gts[0:nt, t, :], b.ident[0:nt, 0:nt], ["gts", "ident"], ["ps7"])
                b.cp("dve", gatesT[:, t * 128:t * 128 + nt], b.ps[7][0:NE, 0:nt], ["ps7"], ["gatesT"])
            k.barrier()

        with ExitStack() as es2:
            sb2 = lambda n, s, d: es2.enter_context(UT(nc, n, s, d))
            stg = [sb2(f"stg{i}", [128, 2048], F32) for i in range(3)]
            w1b = [sb2(f"w1b{i}", [128, 16, 2, 128], BF16) for i in range(2)]
            w2b = [sb2(f"w2b{i}", [128, 4, 512], BF16) for i in range(2)]
            actT = sb2("actT", [128, 4, NOWN], BF16)
            Gsb = sb2("Gsb", [128, NOWN], F32)
            tms = [[sb2(f"e{a}_{i}", [128, 512], F32) for a in range(3)] for i in range(2)]
            sctr = [0]
            w1ctr = [0]
            w2ctr = [0]
            ectr = [0]
            yctr = [0]

            def stage():
                i = sctr[0] % 3
                sctr[0] += 1
                return stg[i], f"stg{i}"

            for e in range(b.n_experts):
                b.ts("dve", gsel[:], gatesT[:], b.ident[0:NE, e:e + 1], None, OP.mult, None, ["gatesT", "ident"], ["gsel"])
                for tg, (c0, n, j) in enumerate(b.groups):
                    b.mm(b.ps[6][:, :n], b.ones_f[0:NE, :], gsel[:, c0:c0 + n], True, True, ["ones_f", "gsel"], ["ps6"])
                    b.cp("act", Gsb[:, c0:c0 + n], b.ps[6][:, :n], ["ps6"], [("Gsb", tg)])
                w1e = w1_d[e].rearrange("(c p) m -> p c m", p=128)
                w2e = w2_d[e].rearrange("(c p) m -> p c m", p=128)
                for qt in range(4):
                    for fcl in range(4):
                        fc = 4 * qt + fcl
                        wi = w1ctr[0] % 2
                        w1ctr[0] += 1
                        for hh in range(2):
                            st, skey = stage()
                            sv = st[:].rearrange("p (c m) -> p c m", c=8)
                            k.dma(sv, w1e[:, hh * 8:(hh + 1) * 8, fc * 256:(fc + 1) * 256], W=[skey])
                            svd = sv.rearrange("p c (f two) -> p c two f", two=2)
                            b.cp("act", w1b[wi][:, hh * 8:(hh + 1) * 8, 0, :], svd[:, :, 0, :], [skey], [f"w1b{wi}"])
                            b.cp("pool", w1b[wi][:, hh * 8:(hh + 1) * 8, 1, :], svd[:, :, 1, :], [skey], [f"w1b{wi}"])
                        for tg, (c0, n, j) in enumerate(b.groups):
                            ei = ectr[0] % 2
                            ectr[0] += 1
                            pg, pl = b.ps[2 * ei], b.ps[2 * ei + 1]
                            kg, kl = f"ps{2 * ei}", f"ps{2 * ei + 1}"
                            for two, (pp, kk) in enumerate(((pg, kg), (pl, kl))):
                                for kc in range(16):
                                    b.mm(pp[:, :n], w1b[wi][:, kc, two, :], hx[:, kc, c0:c0 + n], kc == 0, kc == 15,
                                         [f"w1b{wi}", ("hx", tg)], [kk])
                            t1, t2, t3 = tms[ei]
                            n1, n2, n3 = (f"e{a}_{ei}" for a in range(3))
                            col = e * 16 + fc
                            b.ts("dve", t1[:, :n], pg[:, :n], b1g[:, col:col + 1], SWIGLU_LIMIT, OP.add, OP.min, [kg, "b1"], [n1])
                            b.act(t2[:, :n], t1[:, :n], AF.Sigmoid, [n1], [n2], scale=SWIGLU_ALPHA)
                            b.act(t3[:, :n], pl[:, :n], AF.Identity, [kl, "b1"], [n3], bias=b1l[:, col:col + 1])
                            b.ts("pool", t3[:, :n], t3[:, :n], SWIGLU_LIMIT, -SWIGLU_LIMIT, OP.min, OP.max, [n3], [n3])
                            b.tt("pool", t2[:, :n], t1[:, :n], t2[:, :n], OP.mult, [n1, n2], [n2])
                            b.tt("pool", t2[:, :n], t2[:, :n], Gsb[:, c0:c0 + n], OP.mult, [n2, ("Gsb", tg)], [n2])
                            b.stt(actT[:, fcl, c0:c0 + n], t3[:, :n], 1.0, t2[:, :n], OP.add, OP.mult, [n2, n3], [("actT", fcl, tg)])
                    for db in range(4):
                        wi = w2ctr[0] % 2
                        w2ctr[0] += 1
                        st, skey = stage()
                        sv = st[:].rearrange("p (c m) -> p c m", c=4)
                        k.dma(sv, w2e[:, 4 * qt:4 * qt + 4, db * 512:(db + 1) * 512], W=[skey])
                        b.cp("act" if db % 2 == 0 else "pool", w2b[wi][:], sv, [skey], [f"w2b{wi}"])
                        for dcl in range(4):
                            dc = 4 * db + dcl
                            for tg, (c0, n, j) in enumerate(b.groups):
                                pb = 4 + yctr[0] % 2
                                yctr[0] += 1
                                for fcl in range(4):
                                    b.mm(b.ps[pb][:, :n], w2b[wi][:, fcl, dcl * 128:(dcl + 1) * 128], actT[:, fcl, c0:c0 + n],
                                         fcl == 0, fcl == 3, [f"w2b{wi}", ("actT", fcl, tg)], [f"ps{pb}"])
                                b.stt(xT[:, dc, c0:c0 + n], b.ps[pb][:, :n], b.mG(1, j, dc), xT[:, dc, c0:c0 + n], OP.mult, OP.add,
                                      [f"ps{pb}", "modT", xkey(dc, tg)], [xkey(dc, tg)])
            k.barrier()
        with ExitStack() as es2:
            b2 = es2.enter_context(UT(nc, "b2", [NE, D], F32))
            k.dma(b2[:], b2_d, W=["b2"])
            it = 0
            for dc in range(16):
                for tg, (c0, n, j) in enumerate(b.groups):
                    pb = 4 + it % 2
                    it += 1
                    b.mm(b.ps[pb][:, :n], b2[:, dc * 128:(dc + 1) * 128], gatesT[:, c0:c0 + n], True, True, ["b2", "gatesT"], [f"ps{pb}"])
                    b.stt(xT[:, dc, c0:c0 + n], b.ps[pb][:, :n], b.mG(1, j, dc), xT[:, dc, c0:c0 + n], OP.mult, OP.add,
                          [f"ps{pb}", "modT", xkey(dc, tg)], [xkey(dc, tg)])
            k.barrier()


def _consts():
    rot = np.zeros((128, 128), np.float32)
    for d in range(128):
        i = d % 64
        if i < 32:
            rot[d, d + 32] = -1.0
        else:
            rot[d, d - 32] = 1.0
    return {
        "c_ident": np.eye(128, dtype=np.float32),
        "c_ones": np.ones((128, 128), np.float32),
        "c_rotT": np.ascontiguousarray(rot.T).astype(ml_dtypes.bfloat16),
        "c_eps": np.full((128, 1), EPS, np.float32),
    }


def _rope_tables():
    m = 32
    inv_freq = (np.float32(10000.0) ** (-np.arange(m, dtype=np.float32) / np.float32(m))).astype(np.float32)
    t = np.arange(S)
    pos = [(t // 64).astype(np.float32), (t % 64).astype(np.float32)]
    cos = np.zeros((128, S), np.float32)
    sin = np.zeros((128, S), np.float32)
    for d in range(128):
        half, f = d // 64, d % 32
        ang = (pos[half] * inv_freq[f]).astype(np.float32)
        cos[d] = np.cos(ang)
        sin[d] = np.sin(ang)
    return cos, sin


_CACHE = {}


def _get_prog(name, fn):
    if name not in _CACHE:
        _CACHE[name] = fn()
    return _CACHE[name]


def _moe_inputs(inp, i):
    return {"router_w": inp["router_w"][i], "router_b": inp["router_b"][i][None, :], "exp_w1": inp["exp_w1"][i],
            "exp_b1": inp["exp_b1"][i], "exp_w2": inp["exp_w2"][i], "exp_b2": inp["exp_b2"][i]}


def layer0_inputs(inp, x, ctx):
    consts = _consts()
    cos, sin = _rope_tables()
    maps = []
    for core in range(8):
        bi, q = core // 4, core % 4
        perm = np.concatenate([np.arange(q * NLAT, (q + 1) * NLAT), np.arange(0, q * NLAT), np.arange((q + 1) * NLAT, S)])
        cperm = np.concatenate([np.arange(q * NCTX, (q + 1) * NCTX), np.arange(0, q * NCTX), np.arange((q + 1) * NCTX, LCTX)])
        m = dict(consts)
        m["xall"] = np.ascontiguousarray(x[bi][perm])
        m["ctxb"] = np.ascontiguousarray(ctx[bi][cperm])
        m["cosT"] = np.ascontiguousarray(cos[:, perm])
        m["sinT"] = np.ascontiguousarray(sin[:, perm])
        m["cvec"] = np.stack([inp["c"][bi], inp["c_ctx"]]).astype(np.float32)
        m["norm_g"] = np.stack([inp["norm_mix_g"][0], inp["norm_ffn_g"][0]])
        m["qk_gain"] = np.stack([inp["a_q_gain"][0], inp["a_k_gain"][0]])
        m["ada_w"] = inp["ada_w"][0]
        m["ada_b"] = inp["ada_b"][0]
        m["w_qkv"] = inp["a_w_qkv"][0]
        m["w_o"] = inp["a_w_o"][0]
        m.update(_moe_inputs(inp, 0))
        maps.append(m)
    return maps


def _bias_table(rel_bias):
    col = np.arange(64)
    cst = np.clip(col - 8, 0, 48)
    cmask = (col[None, :] >= cst[:, None]) & (col[None, :] < cst[:, None] + 16)
    dcidx = np.clip(col[None, :] - col[:, None], -15, 15) + 15
    g = rel_bias[:, :, dcidx]
    g = np.where(cmask[None, None], g, np.float32(NEG)).astype(np.float32)
    return np.ascontiguousarray(g.transpose(0, 3, 1, 2))


def _row_bias(q):
    R0 = 16 * q
    rb = np.full((128, 16, 12), NEG, np.float32)
    for j in range(16):
        r = R0 + j
        r0 = min(max(r - 4, 0), 56)
        for idx, lr in enumerate(_lrs(j)):
            gr = R0 - 4 + lr
            if 0 <= gr <= 63 and r0 <= gr < r0 + 8:
                rb[:, j, idx] = 0.0
    return rb


def layer1_inputs(inp, x1, ctx1):
    consts = _consts()
    tblh = _bias_table(inp["b_rel_bias"][0])
    maps = []
    for core in range(8):
        bi, q = core // 4, core % 4
        m = dict(consts)
        if x1 is not None:
            m["xown"] = np.ascontiguousarray(x1[bi, q * NLAT:(q + 1) * NLAT])
            oth = np.zeros((NHALO_T + NHALO_B + LCTX, D), np.float32)
            lo = q * NLAT - NHALO_T
            if lo >= 0:
                oth[0:NHALO_T] = x1[bi, lo:lo + NHALO_T]
            hi = (q + 1) * NLAT
            if hi + NHALO_B <= S:
                oth[NHALO_T:NHALO_T + NHALO_B] = x1[bi, hi:hi + NHALO_B]
            oth[NHALO_T + NHALO_B:] = ctx1[bi]
            m["xoth"] = oth
        m["rowbias"] = _row_bias(q)
        m["bias_tbl"] = tblh
        m["cvec"] = np.stack([inp["c"][bi], inp["c_ctx"]]).astype(np.float32)
        m["norm_g"] = np.stack([inp["norm_mix_g"][1], inp["norm_ffn_g"][1]])
        m["qk_gain"] = np.stack([inp["b_q_gain"][0], inp["b_k_gain"][0]])
        m["ada_w"] = inp["ada_w"][1]
        m["ada_b"] = inp["ada_b"][1]
        m["w_qkv"] = inp["b_w_qkv"][0]
        m["w_o"] = inp["b_w_o"][0]
        m.update(_moe_inputs(inp, 1))
        maps.append(m)
    return maps


def _gather_idx(core):
    bi, q = core // 4, core % 4
    up = core - 1 if q > 0 else core
    dn = core + 1 if q < 3 else core
    top = up * NEDGE + 192 + np.arange(256)
    bot = dn * NEDGE + np.arange(192)
    cx = np.concatenate([(4 * bi + cq) * NEDGE + 448 + np.arange(64) for cq in range(4)])
    allr = np.concatenate([top, bot, cx])
    tiles = [(0, 128), (128, 128), (256, 128), (384, 64), (448, 128), (576, 128)]
    g = np.zeros((128, 6), np.int32)
    for t, (r0, n) in enumerate(tiles):
        g[:n, t] = allr[r0:r0 + n]
    return g


def fused_inputs(inp):
    l0 = layer0_inputs(inp, inp["x"], inp["ctx"])
    l1 = layer1_inputs(inp, None, None)
    maps = []
    for core in range(8):
        m = {}
        for kk, v in l0[core].items():
            m[kk + "_l0" if kk in Builder.LAYER_INPUTS else kk] = v
        for kk, v in l1[core].items():
            if kk in Builder.LAYER_INPUTS:
                m[kk + "_l1"] = v
            elif kk in ("rowbias", "bias_tbl"):
                m[kk] = v
        m["gidx"] = _gather_idx(core)
        maps.append(m)
    return maps


def _run(bld, maps):
    maps = [{kk: np.ascontiguousarray(v) for kk, v in m.items() if kk in bld.din} for m in maps]
    return run_bass_kernel_spmd(bld.nc, maps, core_ids=list(range(8))).results


def kernel(**inputs):
    inp = {kk: np.asarray(v) for kk, v in inputs.items()}
    b0 = _get_prog("l0", build_layer0)
    r0 = _run(b0, layer0_inputs(inp, inp["x"], inp["ctx"]))
    x1 = np.zeros((2, S, D), np.float32)
    ctx1 = np.zeros((2, LCTX, D), np.float32)
    for core in range(8):
        bi, q = core // 4, core % 4
        o = r0[core]["x1"]
        x1[bi, q * NLAT:(q + 1) * NLAT] = o[:NLAT]
        ctx1[bi, q * NCTX:(q + 1) * NCTX] = o[NLAT:]
    b1 = _get_prog("l1", build_layer1)
    r1 = _run(b1, layer1_inputs(inp, x1, ctx1))
    out = np.zeros((2, S, D), np.float32)
    for core in range(8):
        bi, q = core // 4, core % 4
        out[bi, q * NLAT:(q + 1) * NLAT] = r1[core]["yout"]
    return out
```

```python
from contextlib import ExitStack

import numpy as np
import ml_dtypes
import concourse.bass as bass
import concourse.mybir as mybir
from concourse.bass_utils import run_bass_kernel_spmd

F32 = mybir.dt.float32
BF16 = mybir.dt.bfloat16
AF = mybir.ActivationFunctionType
OP = mybir.AluOpType

D = 2048
NCH = 16
S = 4096
LCTX = 256
NLAT = 1024
NCTX = 64
NE = 32
EPS = 1e-6
ATT_SCALE = 128 ** -0.5
SWIGLU_ALPHA = 1.702
SWIGLU_LIMIT = 7.0
SEM_EPOCH = 12000


_UID = [0]


def UT(nc, name, shape, dt):
    _UID[0] += 1
    return nc.sbuf_tensor(f"{name}_u{_UID[0]}", shape, dt)


class Ctx:
    def __init__(self, nc, n_dma_sems=16):
        self.nc = nc
        self.eng = {"pe": nc.tensor, "act": nc.scalar, "dve": nc.vector,
                    "pool": nc.gpsimd, "sp": nc.sync}
        self.sem = {e: nc.alloc_semaphore(name=f"s_{e}_0") for e in ("pe", "act", "dve", "pool")}
        self.cnt = {e: 0 for e in self.sem}
        self.epoch = {e: 0 for e in self.sem}
        self.dsem = [nc.alloc_semaphore(name=f"d_{i}") for i in range(n_dma_sems)]
        self.dcnt = [0] * n_dma_sems
        self.drr = 0
        self.waited = {e: {} for e in self.eng}
        self.lastw = {}
        self.readers = {}
        self.n_inst = 0

    def _wait(self, e, tok):
        sem, val, key = tok
        if self.waited[e].get(key, 0) >= val:
            return
        self.eng[e].wait_ge(sem, val)
        self.waited[e][key] = val

    def _deps(self, e, R, W):
        for b in R:
            for t in self.lastw.get(b, {}).values():
                if not (e == "pe" and t[2][0] == "pe"):
                    self._wait(e, t)
        for b in W:
            for t in self.lastw.get(b, {}).values():
                if not (e == "pe" and t[2][0] == "pe"):
                    self._wait(e, t)
            for t in self.readers.get(b, {}).values():
                if not (e == "pe" and t[2][0] == "pe"):
                    self._wait(e, t)

    def _record(self, tok, R, W):
        for b in W:
            self.lastw.setdefault(b, {})[tok[2]] = tok
        for b in R:
            self.readers.setdefault(b, {})[tok[2]] = tok

    def op(self, e, fn, R=(), W=()):
        self._deps(e, R, W)
        if self.cnt[e] >= SEM_EPOCH:
            self.epoch[e] += 1
            self.sem[e] = self.nc.alloc_semaphore(name=f"s_{e}_{self.epoch[e]}")
            self.cnt[e] = 0
        inst = fn()
        self.cnt[e] += 1
        inst.then_inc(self.sem[e], 1)
        tok = (self.sem[e], self.cnt[e], (e, self.epoch[e]))
        self._record(tok, R, W)
        self.n_inst += 1
        return tok

    def dma(self, out, in_, R=(), W=(), q="sp", **kw):
        i = self.drr
        self.drr = (self.drr + 1) % len(self.dsem)
        self._deps(q, R, W)
        if self.dcnt[i] > 0:
            self._wait(q, (self.dsem[i], 16 * self.dcnt[i], ("d", i)))
        self.eng[q].dma_start(out=out, in_=in_, **kw).then_inc(self.dsem[i], 16)
        self.dcnt[i] += 1
        tok = (self.dsem[i], 16 * self.dcnt[i], ("d", i))
        self._record(tok, R, W)
        self.n_inst += 1
        return tok

    def dma_custom(self, q, fn, R=(), W=()):
        i = self.drr
        self.drr = (self.drr + 1) % len(self.dsem)
        self._deps(q, R, W)
        if self.dcnt[i] > 0:
            self._wait(q, (self.dsem[i], 16 * self.dcnt[i], ("d", i)))
        fn(self.eng[q]).then_inc(self.dsem[i], 16)
        self.dcnt[i] += 1
        tok = (self.dsem[i], 16 * self.dcnt[i], ("d", i))
        self._record(tok, R, W)
        self.n_inst += 1
        return tok

    def wait_all(self, e):
        for i, s in enumerate(self.dsem):
            if self.dcnt[i]:
                self._wait(e, (s, 16 * self.dcnt[i], ("d", i)))
        for kk, s in self.sem.items():
            if self.cnt[kk]:
                self._wait(e, (s, self.cnt[kk], (kk, self.epoch[kk])))

    def barrier(self):
        for e in self.eng:
            self.wait_all(e)


class Builder:
    def __init__(self, layer, n_experts=NE, debug_stop=None):
        self.layer = layer
        self.n_experts = n_experts
        self.debug_stop = debug_stop
        nc = self.nc = bass.Bass("TRN2", target_bir_lowering=False)
        self.k = Ctx(nc)
        self.T, self.V, self.A, self.P = nc.tensor, nc.vector, nc.scalar, nc.gpsimd
        self.ps = [nc.alloc_psum_tensor(f"ps{i}", [128, 512], F32) for i in range(8)]
        self.din = {}
        self.sfx = ""
        self.NOWN = NLAT + NCTX if layer == 0 else NLAT
        self.groups = [(0, 512, 0), (512, 512, 0)] + ([(1024, 64, 1)] if layer == 0 else [])

    LAYER_INPUTS = ("norm_g", "qk_gain", "ada_w", "ada_b", "w_qkv", "w_o", "router_w", "router_b",
                    "exp_w1", "exp_b1", "exp_w2", "exp_b2")

    def inp(self, name, shape, dt=F32):
        if name in self.LAYER_INPUTS:
            name = name + self.sfx
        if name not in self.din:
            self.din[name] = self.nc.dram_tensor(name, list(shape), dt, kind="ExternalInput")
        return self.din[name].ap()

    def set_layer(self, layer):
        self.layer = layer
        self.NOWN = NLAT + NCTX if layer == 0 else NLAT
        self.groups = [(0, 512, 0), (512, 512, 0)] + ([(1024, 64, 1)] if layer == 0 else [])

    def mm(self, out, lhsT, rhs, start, stop, R, W, **kw):
        return self.k.op("pe", lambda: self.T.matmul(out, lhsT=lhsT, rhs=rhs, start=start, stop=stop, **kw), R, W)

    def tr(self, out, in_, ident, R, W):
        return self.k.op("pe", lambda: self.T.transpose(out, in_, ident), R, W)

    def act(self, out, in_, func, R, W, **kw):
        return self.k.op("act", lambda: self.A.activation(out, in_, func, **kw), R, W)

    def ts(self, e, out, in0, s1, s2, op0, op1, R, W):
        eng = self.V if e == "dve" else self.P
        if op1 is None:
            return self.k.op(e, lambda: eng.tensor_scalar(out, in0, s1, None, op0), R, W)
        return self.k.op(e, lambda: eng.tensor_scalar(out, in0, s1, s2, op0, op1), R, W)

    def tt(self, e, out, in0, in1, op, R, W):
        eng = self.V if e == "dve" else self.P
        return self.k.op(e, lambda: eng.tensor_tensor(out, in0, in1, op), R, W)

    def stt(self, out, in0, scalar, in1, op0, op1, R, W):
        return self.k.op("dve", lambda: self.V.scalar_tensor_tensor(out, in0, scalar, in1, op0, op1), R, W)

    def cp(self, e, out, in_, R, W):
        if e == "act":
            return self.act(out, in_, AF.Copy, R, W)
        eng = self.V if e == "dve" else self.P
        return self.k.op(e, lambda: eng.tensor_copy(out, in_), R, W)

    def recip(self, out, in_, R, W):
        return self.k.op("dve", lambda: self.V.reciprocal(out, in_), R, W)

    def setup_consts(self, es):
        nc, k = self.nc, self.k
        sb = lambda n, s, d: es.enter_context(UT(nc, n, s, d))
        self.ident = sb("ident", [128, 128], F32)
        self.ones_f = sb("ones_f", [128, 128], F32)
        self.ones_b = sb("ones_b", [128, 128], BF16)
        self.rotT = sb("rotT", [128, 128], BF16)
        self.epsc = sb("epsc", [128, 1], F32)
        k.dma(self.ident[:], self.inp("c_ident", [128, 128]), W=["ident"])
        k.dma(self.ones_f[:], self.inp("c_ones", [128, 128]), W=["ones_f"])
        k.dma(self.rotT[:], self.inp("c_rotT", [128, 128], BF16), W=["rotT"])
        k.dma(self.epsc[:], self.inp("c_eps", [128, 1]), W=["epsc"])
        self.cp("dve", self.ones_b[:], self.ones_f[:], ["ones_f"], ["ones_b"])

    def load_cols(self, dst, src2d, n, key, tmp, ps, pskey):
        self.k.dma(tmp[0:n, :], src2d, W=["lc_tmp"])
        self.tr(ps[:, 0:n], tmp[0:n, :], self.ident[0:n, 0:n], ["lc_tmp", "ident"], [pskey])
        self.cp("dve", dst, ps[:, 0:n], [pskey], [key])

    def setup_vectors(self, es, es_tmp):
        nc, k = self.nc, self.k
        sb = lambda n, s, d: es.enter_context(UT(nc, n, s, d))
        sbt = lambda n, s, d: es_tmp.enter_context(UT(nc, n, s, d))
        self.gT = sb("gT", [128, 2, 16], F32)
        self.qkg = sb("qkg", [128, 2], F32)
        self.modT = sb("modT", [128, 96, 2], F32)
        self.modA = sb("modA", [128, 2, 2, 16], F32)
        tmp = sbt("lc_tmp", [128, 128], F32)
        ps = self.ps[7]
        gsrc = self.inp("norm_g", [2, D])
        self.load_cols(self.gT[:].rearrange("p a c -> p (a c)"), gsrc.rearrange("a (c p) -> (a c) p", p=128), 32,
                       "gT", tmp, ps, "ps7")
        self.load_cols(self.qkg[:], self.inp("qk_gain", [2, 128]), 2, "qkg", tmp, ps, "ps7")
        cT = sbt("cT", [128, 2, 16], F32)
        self.load_cols(cT[:].rearrange("p a c -> p (a c)"), self.inp("cvec", [2, D]).rearrange("a (c p) -> (a c) p", p=128),
                       32, "cT", tmp, ps, "ps7")
        scT = sbt("scT", [128, 2, 16], F32)
        self.act(scT[:], cT[:], AF.Silu, ["cT"], ["scT"])
        abT = sbt("abT", [128, 96], F32)
        self.load_cols(abT[:], self.inp("ada_b", [6 * D]).rearrange("(c p) -> c p", p=128), 96, "abT", tmp, ps, "ps7")
        adaw = self.inp("ada_w", [D, 6 * D]).rearrange("(c p) m -> p c m", p=128)
        wst = [sbt(f"adaw{i}", [128, 16, 512], F32) for i in range(2)]
        mps = self.ps[6]
        for blk in range(24):
            w = wst[blk % 2]
            wk = f"adaw{blk % 2}"
            for h in range(2):
                k.dma(w[:, h * 8:(h + 1) * 8, :], adaw[:, h * 8:(h + 1) * 8, blk * 512:(blk + 1) * 512], W=[wk])
            for mc in range(4):
                m = blk * 4 + mc
                for kc in range(16):
                    self.mm(mps[:, 2 * m:2 * m + 2], w[:, kc, mc * 128:(mc + 1) * 128], scT[:, :, kc],
                            kc == 0, kc == 15, [wk, "scT"], ["ps6"])
        for j in range(2):
            self.tt("dve", self.modT[:, :, j], mps[:, 0:192].rearrange("p (m j) -> p m j", j=2)[:, :, j], abT[:],
                    OP.add, ["ps6", "abT"], ["modT"])
        for n in range(2):
            for j in range(2):
                sc = self.modT[:, (3 * n + 1) * 16:(3 * n + 2) * 16, j]
                self.stt(self.modA[:, n, j, :], sc, 1.0, self.gT[:, n, :], OP.add, OP.mult, ["modT", "gT"], ["modA"])

    def mA(self, n, j, c):
        return self.modA[:, n, j, c:c + 1]

    def mB(self, n, j, c):
        return self.modT[:, 3 * n * 16 + c, j:j + 1]

    def mG(self, n, j, c):
        return self.modT[:, (3 * n + 2) * 16 + c, j:j + 1]

    def norm_mod(self, src, skey, ncols, n_idx, j, dst_fn, tmps, ps, pskey, f32_fn=None, post_fn=None):
        sq, rs, tmp = tmps["sq"], tmps["rs"], tmps["tmp"]
        for kc in range(16):
            i = kc % 2
            self.act(sq[i][:, :ncols], src(kc), AF.Square, [skey(kc)], [f"sq{i}"])
            self.mm(ps[:, :ncols], self.ones_f[:], sq[i][:, :ncols], kc == 0, kc == 15, [f"sq{i}", "ones_f"], [pskey])
        self.act(rs[:, :ncols], ps[:, :ncols], AF.Sqrt, [pskey, "epsc"], ["rs"], scale=1.0 / D, bias=self.epsc[:, 0:1])
        self.recip(rs[:, :ncols], rs[:, :ncols], ["rs"], ["rs"])
        for kc in range(16):
            i = kc % 2
            self.tt("dve", tmp[i][:, :ncols], src(kc), rs[:, :ncols], OP.mult, [skey(kc), "rs"], [f"tmp{i}"])
            out, okey = dst_fn(kc)
            if f32_fn is None:
                self.act(out, tmp[i][:, :ncols], AF.Identity, [f"tmp{i}", "modA", "modT"], [okey],
                         scale=self.mA(n_idx, j, kc), bias=self.mB(n_idx, j, kc))
            else:
                h32, hkey = f32_fn(kc)
                self.act(h32, tmp[i][:, :ncols], AF.Identity, [f"tmp{i}", "modA", "modT"], [hkey],
                         scale=self.mA(n_idx, j, kc), bias=self.mB(n_idx, j, kc))
                self.cp("pool", out, h32, [hkey], [okey])
            if post_fn is not None:
                post_fn(kc)

    def qk_finish(self, pps, ppkey, ncols, gain_col, rope, dst, dkey, tm, cos=None, sin=None, cskey=None):
        ps_ss, ps_rot = self.ps[4], self.ps[5]
        self.act(tm["ksq"][:, :ncols], pps, AF.Square, [ppkey], ["ksq"])
        self.mm(ps_ss[:, :ncols], self.ones_f[:], tm["ksq"][:, :ncols], True, True, ["ksq", "ones_f"], ["ps4"])
        self.act(tm["krs"][:, :ncols], ps_ss[:, :ncols], AF.Sqrt, ["ps4", "epsc"], ["krs"], scale=1.0 / 128, bias=self.epsc[:, 0:1])
        self.recip(tm["krs"][:, :ncols], tm["krs"][:, :ncols], ["krs"], ["krs"])
        if not rope:
            self.stt(dst, pps, self.qkg[:, gain_col:gain_col + 1], tm["krs"][:, :ncols], OP.mult, OP.mult,
                     [ppkey, "krs", "qkg"], [dkey])
            return
        self.stt(tm["kn"][:, :ncols], pps, self.qkg[:, gain_col:gain_col + 1], tm["krs"][:, :ncols], OP.mult, OP.mult,
                 [ppkey, "krs", "qkg"], ["kn"])
        self.mm(ps_rot[:, :ncols], self.rotT[:], tm["kn"][:, :ncols], True, True, ["kn", "rotT"], ["ps5"])
        self.tt("pool", tm["t1"][:, :ncols], tm["kn"][:, :ncols], cos, OP.mult, ["kn", cskey], ["t1"])
        self.tt("dve", tm["t2"][:, :ncols], ps_rot[:, :ncols], sin, OP.mult, ["ps5", cskey], ["t2"])
        self.tt("pool", dst, tm["t1"][:, :ncols], tm["t2"][:, :ncols], OP.add, ["t1", "t2"], [dkey])

    def load_w(self, src_ap, stage, skey, dst, dkey, cast_eng="act", deint=False):
        self.k.dma(stage, src_ap, W=[skey])
        if not deint:
            self.cp(cast_eng, dst, stage, [skey], [dkey])
        else:
            sv = stage.rearrange("p c (f two) -> p c two f", two=2)
            self.cp("act", dst[:, :, 0, :], sv[:, :, 0, :], [skey], [dkey])
            self.cp("pool", dst[:, :, 1, :], sv[:, :, 1, :], [skey], [dkey])


def build_layer0(n_experts=NE, debug_stop=None):
    b = Builder(0, n_experts, debug_stop)
    nc, k = b.nc, b.k
    NOWN = b.NOWN
    es_all = ExitStack()
    sbA = lambda n, s, d: es_all.enter_context(UT(nc, n, s, d))
    b.setup_consts(es_all)
    with ExitStack() as es_tmp:
        b.setup_vectors(es_all, es_tmp)
        k.barrier()

    xT = sbA("xT", [128, 16, NOWN], F32)
    xkey = lambda kc, tg: ("xT", kc, tg)
    layer0_body(b, xT, xkey)
    x1 = nc.dram_tensor("x1", [NOWN, D], F32, kind="ExternalOutput").ap()
    store_tokens(b, xT, xkey, x1, NOWN)
    return b


def layer0_body(b, xT, xkey):
    nc, k = b.nc, b.k
    NOWN = b.NOWN
    xall = b.inp("xall", [S, D])
    ctxb = b.inp("ctxb", [LCTX, D])
    cosd = b.inp("cosT", [128, S])
    sind = b.inp("sinT", [128, S])
    wqkv = b.inp("w_qkv", [D, 3072]).rearrange("(c p) m -> p c m", p=128)
    wo = b.inp("w_o", [D, D]).rearrange("(c p) m -> p c m", p=128)
    NKEY = S + LCTX
    kT_d = nc.dram_tensor("kT0_d", [4, 128, NKEY], BF16, kind="Internal").ap()
    v_d = nc.dram_tensor("v0_d", [NKEY, 512], BF16, kind="Internal").ap()

    with ExitStack() as es:
        sb = lambda n, s, d: es.enter_context(UT(nc, n, s, d))
        wkv = sb("wkv", [128, 16, 1024], BF16)
        xs = [sb(f"xs{i}", [128, D], F32) for i in range(2)]
        xTg = [sb(f"xTg{i}", [128, 16, 256], F32) for i in range(2)]
        hxg = [sb(f"hxg{i}", [128, 16, 256], BF16) for i in range(2)]
        tm = {"sq": [sb(f"sq{i}", [128, 256], F32) for i in range(2)], "rs": sb("rs", [128, 256], F32),
              "tmp": [sb(f"tmp{i}", [128, 256], F32) for i in range(2)],
              "ksq": sb("ksq", [128, 256], F32), "krs": sb("krs", [128, 256], F32), "kn": sb("kn", [128, 256], BF16),
              "t1": sb("t1", [128, 256], F32), "t2": sb("t2", [128, 256], F32)}
        cs = [sb(f"cs{i}", [128, 2, 256], F32) for i in range(2)]
        kout = [sb(f"kout{i}", [128, 4, 256], BF16) for i in range(2)]
        vout = [sb(f"vout{i}", [128, 2, 512], BF16) for i in range(2)]
        for q in range(4):
            b.load_w(wqkv[:, :, 2048 + q * 256:2048 + (q + 1) * 256], xTg[q % 2][:], f"xTg{q % 2}",
                     wkv[:, :, q * 256:(q + 1) * 256], "wkv")
        n_groups = 17
        ev = 0
        for g in range(n_groups):
            is_ctx = g == 16
            own = g < 4
            gi = g % 2
            j = 1 if is_ctx else 0
            if not is_ctx:
                k.dma(cs[gi][:, 0, :], cosd[:, g * 256:(g + 1) * 256], W=[f"cs{gi}"])
                k.dma(cs[gi][:, 1, :], sind[:, g * 256:(g + 1) * 256], W=[f"cs{gi}"])
            for t2 in range(2):
                xi = (2 * g + t2) % 2
                rows = ctxb[t2 * 128:(t2 + 1) * 128, :] if is_ctx else xall[g * 256 + t2 * 128:g * 256 + (t2 + 1) * 128, :]
                k.dma(xs[xi][:], rows, W=[f"xs{xi}"])
                for m in range(4):
                    pb = ev % 2
                    for jj in range(4):
                        kc = 4 * m + jj
                        b.tr(b.ps[pb][:, jj * 128:(jj + 1) * 128], xs[xi][:, kc * 128:(kc + 1) * 128], b.ident[:],
                             [f"xs{xi}", "ident"], [f"ps{pb}"])
                    b.cp("act" if ev % 2 == 0 else "dve", xTg[gi][:, 4 * m:4 * m + 4, t2 * 128:(t2 + 1) * 128],
                         b.ps[pb][:].rearrange("p (c n) -> p c n", c=4), [f"ps{pb}"], [f"xTg{gi}"])
                    ev += 1
            if own:
                for h in range(2):
                    b.cp("pool", xT[:, h * 8:(h + 1) * 8, g * 256:(g + 1) * 256], xTg[gi][:, h * 8:(h + 1) * 8, :], [f"xTg{gi}"],
                         [xkey(kc, g // 2) for kc in range(h * 8, (h + 1) * 8)])
            if is_ctx:
                b.cp("pool", xT[:, :, 1024:1088], xTg[gi][:, :, 0:64], [f"xTg{gi}"], [xkey(kc, 2) for kc in range(16)])
            b.norm_mod(lambda kc: xTg[gi][:, kc, :], lambda kc: f"xTg{gi}", 256, 0, j,
                       lambda kc: (hxg[gi][:, kc, :], f"hxg{gi}"), tm, b.ps[2], "ps2")
            for kvh in range(4):
                for kc in range(16):
                    b.mm(b.ps[3][:, :256], wkv[:, kc, kvh * 128:(kvh + 1) * 128], hxg[gi][:, kc, :], kc == 0, kc == 15,
                         ["wkv", f"hxg{gi}"], ["ps3"])
                b.qk_finish(b.ps[3][:, :256], "ps3", 256, 1, not is_ctx, kout[gi][:, kvh, :], f"kout{gi}", tm,
                            cs[gi][:, 0, :], cs[gi][:, 1, :], f"cs{gi}")
            k.dma(kT_d.rearrange("h p t -> p h t")[:, :, g * 256:(g + 1) * 256], kout[gi][:], R=[f"kout{gi}"], W=["kT_d"])
            for t2 in range(2):
                pv = 6 + t2
                for kc in range(16):
                    b.mm(b.ps[pv][:, :], hxg[gi][:, kc, t2 * 128:(t2 + 1) * 128], wkv[:, kc, 512:1024], kc == 0, kc == 15,
                         ["wkv", f"hxg{gi}"], [f"ps{pv}"])
                b.cp("act", vout[gi][:, t2, :], b.ps[pv][:, :], [f"ps{pv}"], [f"vout{gi}"])
            k.dma(v_d[g * 256:(g + 1) * 256, :].rearrange("(t p) c -> p t c", p=128), vout[gi][:], R=[f"vout{gi}"], W=["v_d"])
        k.barrier()

    es_q = ExitStack()
    QT = es_q.enter_context(UT(nc, "QT", [128, 16, NOWN], BF16))
    with ExitStack() as es:
        sb = lambda n, s, d: es.enter_context(UT(nc, n, s, d))
        hx = sb("hx", [128, 16, NOWN], BF16)
        tm = {"sq": [sb(f"sq{i}", [128, 512], F32) for i in range(2)], "rs": sb("rs", [128, 512], F32),
              "tmp": [sb(f"tmp{i}", [128, 512], F32) for i in range(2)],
              "ksq": sb("ksq", [128, 512], F32), "krs": sb("krs", [128, 512], F32), "kn": sb("kn", [128, 512], BF16),
              "t1": sb("t1", [128, 512], F32), "t2": sb("t2", [128, 512], F32)}
        csq = sb("csq", [128, 2, NLAT], F32)
        wst = [sb("wst0", [128, 16, 256], F32)] * 2
        wq = [sb(f"wq{i}", [128, 16, 256], BF16) for i in range(2)]
        k.dma(csq[:, 0, :], cosd[:, 0:NLAT], W=["csq"])
        k.dma(csq[:, 1, :], sind[:, 0:NLAT], W=["csq"])
        for tg, (c0, n, j) in enumerate(b.groups):
            b.norm_mod(lambda kc: xT[:, kc, c0:c0 + n], lambda kc: xkey(kc, tg), n, 0, j,
                       lambda kc: (hx[:, kc, c0:c0 + n], ("hx", tg)), tm, b.ps[2], "ps2")
        for hb in range(8):
            wi = hb % 2
            b.load_w(wqkv[:, :, hb * 256:(hb + 1) * 256], wst[wi][:], "wst0", wq[wi][:], f"wq{wi}")
            for hh in range(2):
                head = 2 * hb + hh
                for tg, (c0, n, j) in enumerate(b.groups):
                    for kc in range(16):
                        b.mm(b.ps[3][:, :n], wq[wi][:, kc, hh * 128:(hh + 1) * 128], hx[:, kc, c0:c0 + n], kc == 0, kc == 15,
                             [f"wq{wi}", ("hx", tg)], ["ps3"])
                    rope = j == 0
                    b.qk_finish(b.ps[3][:, :n], "ps3", n, 0, rope, QT[:, head, c0:c0 + n], ("QT", head, tg), tm,
                                csq[:, 0, c0:c0 + n] if rope else None, csq[:, 1, c0:c0 + n] if rope else None, "csq")
        k.barrier()

    with ExitStack() as es:
        sb = lambda n, s, d: es.enter_context(UT(nc, n, s, d))
        NKT = NKEY // 128
        kts = [sb(f"kts{i}", [128, NKEY], BF16) for i in range(2)]
        vs = [sb(f"vs{i}", [128, NKT, 128], BF16) for i in range(2)]
        pT = [sb(f"pT{i}", [128, 512], BF16) for i in range(3)]
        rinv = sb("rinv", [128, 512], F32)
        it = 0
        for kvh in range(4):
            ki = kvh % 2
            k.dma(kts[ki][:], kT_d[kvh], R=["kT_d"], W=[f"kts{ki}"])
            k.dma(vs[ki][:], v_d.rearrange("(t p) c -> p t c", p=128)[:, :, kvh * 128:(kvh + 1) * 128], R=["v_d"], W=[f"vs{ki}"])
            for h in range(4):
                head = kvh * 4 + h
                for tg, (c0, n, j) in enumerate(b.groups):
                    tiles = list(range(NKT)) if j == 0 else [32, 33]
                    pO, pS = b.ps[4 + it % 2], b.ps[6 + it % 2]
                    kO, kS = f"ps{4 + it % 2}", f"ps{6 + it % 2}"
                    it += 1
                    q_ap = QT[:, head, c0:c0 + n]
                    qkey = ("QT", head, tg)

                    def s_mm(idx):
                        kt = tiles[idx]
                        b.mm(b.ps[idx % 2][:, :n], kts[ki][:, kt * 128:(kt + 1) * 128], q_ap, True, True,
                             [f"kts{ki}", qkey], [f"ps{idx % 2}"])
                    s_mm(0)
                    for idx, kt in enumerate(tiles):
                        if idx + 1 < len(tiles):
                            s_mm(idx + 1)
                        pi = idx % 3
                        b.act(pT[pi][:, :n], b.ps[idx % 2][:, :n], AF.Exp, [f"ps{idx % 2}"], [f"pT{pi}"], scale=ATT_SCALE)
                        b.mm(pO[:, :n], vs[ki][:, kt, :], pT[pi][:, :n], idx == 0, idx == len(tiles) - 1, [f"vs{ki}", f"pT{pi}"], [kO])
                        b.mm(pS[:, :n], b.ones_b[:], pT[pi][:, :n], idx == 0, idx == len(tiles) - 1, ["ones_b", f"pT{pi}"], [kS])
                    b.recip(rinv[:, :n], pS[:, :n], [kS], ["rinv"])
                    b.tt("dve", q_ap, pO[:, :n], rinv[:, :n], OP.mult, [kO, "rinv"], [qkey])
        k.barrier()

    with ExitStack() as es:
        sb = lambda n, s, d: es.enter_context(UT(nc, n, s, d))
        wst = [sb(f"wst{i}", [128, 16, 256], F32) for i in range(2)]
        wob = [sb(f"wob{i}", [128, 16, 256], BF16) for i in range(2)]
        it = 0
        for db in range(8):
            wi = db % 2
            b.load_w(wo[:, :, db * 256:(db + 1) * 256], wst[wi][:], f"wst{wi}", wob[wi][:], f"wob{wi}")
            for dcl in range(2):
                dc = 2 * db + dcl
                for tg, (c0, n, j) in enumerate(b.groups):
                    pb = it % 2
                    it += 1
                    for hc in range(16):
                        b.mm(b.ps[pb][:, :n], wob[wi][:, hc, dcl * 128:(dcl + 1) * 128], QT[:, hc, c0:c0 + n], hc == 0, hc == 15,
                             [f"wob{wi}", ("QT", hc, tg)], [f"ps{pb}"])
                    b.stt(xT[:, dc, c0:c0 + n], b.ps[pb][:, :n], b.mG(0, j, dc), xT[:, dc, c0:c0 + n], OP.mult, OP.add,
                          [f"ps{pb}", "modT", xkey(dc, tg)], [xkey(dc, tg)])
        k.barrier()

    es_q.close()
    if not (b.debug_stop and "mix" in b.debug_stop):
        moe(b, xT, xkey, None)


NHALO_T, NHALO_B = 256, 192
NK1 = NLAT + NHALO_T + NHALO_B + LCTX
NEG = -30000.0


def _kcol(lr):
    if lr < 4:
        return NLAT + lr * 64
    if lr < 20:
        return (lr - 4) * 64
    return NLAT + NHALO_T + (lr - 20) * 64


def _lrs(j):
    if j < 4:
        return list(range(j, 12))
    if j <= 12:
        return list(range(j, j + 8))
    return list(range(12, j + 8))


def build_layer1(n_experts=NE, debug_stop=None):
    b = Builder(1, n_experts, debug_stop)
    nc, k = b.nc, b.k
    es_all = ExitStack()
    sbA = lambda n, s, d: es_all.enter_context(UT(nc, n, s, d))
    b.setup_consts(es_all)
    rvb = sbA("rvb", [128, 16, 12], F32)
    k.dma(rvb[:], b.inp("rowbias", [128, 16, 12]), W=["rvb"])
    with ExitStack() as es_tmp:
        b.setup_vectors(es_all, es_tmp)
        k.barrier()
    xT = sbA("xT", [128, 16, NLAT], F32)
    xkey = lambda kc, tg: ("xT", kc, tg)
    layer1_body(b, xT, xkey, rvb)
    yout = nc.dram_tensor("yout", [NLAT, D], F32, kind="ExternalOutput").ap()
    store_tokens(b, xT, xkey, yout, NLAT)
    return b


def layer1_body(b, xT, xkey, rvb, gather=None):
    nc, k = b.nc, b.k
    if gather is None:
        xown = b.inp("xown", [NLAT, D])
        xoth = b.inp("xoth", [NHALO_T + NHALO_B + LCTX, D])
    wqkv = b.inp("w_qkv", [D, 3 * D]).rearrange("(c p) m -> p c m", p=128)
    wo = b.inp("w_o", [D, D]).rearrange("(c p) m -> p c m", p=128)
    tbl_d = b.inp("bias_tbl", [16, 64, 15, 64])
    kT_d = nc.dram_tensor("kT1_d", [16, 128, NK1], BF16, kind="Internal").ap()
    v_d = nc.dram_tensor("v1_d", [NK1, D], BF16, kind="Internal").ap()
    es_q = ExitStack()
    QT = es_q.enter_context(UT(nc, "QT", [128, 16, NLAT], BF16))

    with ExitStack() as es:
        sb = lambda n, s, d: es.enter_context(UT(nc, n, s, d))
        hx = sb("hx", [128, 16, NLAT], BF16)
        xs = [sb(f"xs{i}", [128, D], F32) for i in range(2)]
        wst = sb("wst0", [128, 16, 256], F32)
        xTg = wst
        wb = [sb(f"wb{i}", [128, 16, 256], BF16) for i in range(2)]
        tm = {"sq": [sb(f"sq{i}", [128, 512], F32) for i in range(2)], "rs": sb("rs", [128, 512], F32),
              "tmp": [sb(f"tmp{i}", [128, 512], F32) for i in range(2)],
              "ksq": sb("ksq", [128, 512], F32), "krs": sb("krs", [128, 512], F32)}
        kout = [sb(f"kout{i}", [128, 512], BF16) for i in range(2)]
        vout = [sb(f"vout{i}", [128, 256], BF16) for i in range(2)]
        ev = [0]

        def load_T(src_rows, n, dst_fn, dkeys):
            xi = (ev[0] // 4) % 2
            if isinstance(src_rows, int):
                gath_d, gidx = gather
                k.dma_custom("pool", lambda eng: eng.indirect_dma_start(
                    out=xs[xi][0:n, :], out_offset=None, in_=gath_d[:, :],
                    in_offset=bass.IndirectOffsetOnAxis(ap=gidx[0:n, src_rows:src_rows + 1], axis=0)),
                    R=["gath_d", "gidx"], W=[f"xs{xi}"])
            else:
                k.dma(xs[xi][0:n, :], src_rows, W=[f"xs{xi}"])
            for m in range(4):
                pb = ev[0] % 2
                ev[0] += 1
                for jj in range(4):
                    kc = 4 * m + jj
                    b.tr(b.ps[pb][:, jj * 128:jj * 128 + n], xs[xi][0:n, kc * 128:(kc + 1) * 128], b.ident[0:n, 0:n],
                         [f"xs{xi}", "ident"], [f"ps{pb}"])
                b.cp("act" if m % 2 == 0 else "dve", dst_fn(m), b.ps[pb][:].rearrange("p (c n) -> p c n", c=4)[:, :, 0:n],
                     [f"ps{pb}"], dkeys(m))

        def qkv_pass(pass_id, ngroups):
            koff = pass_id * NLAT
            sections = ([(0, 0)] if pass_id == 0 else []) + [(1, D), (2, 2 * D)]
            wctr = 0
            oc = 0
            for kind, woff in sections:
                for blk in range(8):
                    wi = wctr % 2
                    wctr += 1
                    b.load_w(wqkv[:, :, woff + blk * 256:woff + (blk + 1) * 256], wst[:], "wst0", wb[wi][:], f"wb{wi}")
                    if kind < 2:
                        for hh in range(2):
                            head = 2 * blk + hh
                            for (c0, n) in ngroups:
                                for kc in range(16):
                                    b.mm(b.ps[3][:, :n], wb[wi][:, kc, hh * 128:(hh + 1) * 128], hx[:, kc, c0:c0 + n], kc == 0, kc == 15,
                                         [f"wb{wi}", "hx"], ["ps3"])
                                if kind == 0:
                                    b.qk_finish(b.ps[3][:, :n], "ps3", n, 0, False, QT[:, head, c0:c0 + n], ("QT", head), tm)
                                else:
                                    oi = oc % 2
                                    oc += 1
                                    b.qk_finish(b.ps[3][:, :n], "ps3", n, 1, False, kout[oi][:, :n], f"kout{oi}", tm)
                                    k.dma(kT_d[head][:, koff + c0:koff + c0 + n], kout[oi][:, :n], R=[f"kout{oi}"], W=["kT_d"])
                    else:
                        for (c0, n) in ngroups:
                            for t0 in range(0, n, 128):
                                nt = min(128, n - t0)
                                oi = oc % 2
                                oc += 1
                                pv = 6 + oi
                                for kc in range(16):
                                    b.mm(b.ps[pv][0:nt, 0:256], hx[:, kc, c0 + t0:c0 + t0 + nt], wb[wi][:, kc, :], kc == 0, kc == 15,
                                         [f"wb{wi}", "hx"], [f"ps{pv}"])
                                b.cp("act", vout[oi][0:nt, :], b.ps[pv][0:nt, 0:256], [f"ps{pv}"], [f"vout{oi}"])
                                k.dma(v_d[koff + c0 + t0:koff + c0 + t0 + nt, blk * 256:(blk + 1) * 256], vout[oi][0:nt, :],
                                      R=[f"vout{oi}"], W=["v_d"])

        for t in range(8 if gather is None else 0):
            load_T(xown[t * 128:(t + 1) * 128, :], 128, lambda m, t=t: xT[:, 4 * m:4 * m + 4, t * 128:(t + 1) * 128],
                   lambda m, t=t: [xkey(kc, t // 4) for kc in range(4 * m, 4 * m + 4)])
        for tg, (c0, n, j) in enumerate(b.groups):
            b.norm_mod(lambda kc: xT[:, kc, c0:c0 + n], lambda kc: xkey(kc, tg), n, 0, 0,
                       lambda kc: (hx[:, kc, c0:c0 + n], "hx"), tm, b.ps[2], "ps2")
        qkv_pass(0, [(0, 512), (512, 512)])
        gt = 0
        for (g0, n, j) in ((0, 256, 0), (256, 192, 0), (448, 256, 1)):
            for t0 in range(0, n, 128):
                nt = min(128, n - t0)
                src = xoth[g0 + t0:g0 + t0 + nt, :] if gather is None else gt
                gt += 1
                load_T(src, nt, lambda m, t0=t0, nt=nt: xTg[:, 4 * m:4 * m + 4, t0:t0 + nt], lambda m: ["wst0"])
            b.norm_mod(lambda kc: xTg[:, kc, 0:n], lambda kc: "wst0", n, 0, j,
                       lambda kc: (hx[:, kc, g0:g0 + n], "hx"), tm, b.ps[2], "ps2")
        qkv_pass(1, [(0, 512), (512, 192)])
        k.barrier()

    with ExitStack() as es:
        sb = lambda n, s, d: es.enter_context(UT(nc, n, s, d))
        kts = [sb(f"kts{i}", [128, NK1], BF16) for i in range(2)]
        vs = [sb(f"vs{i}", [64, 23, 128], BF16) for i in range(2)]
        vsc = [sb(f"vsc{i}", [128, 2, 128], BF16) for i in range(2)]
        tbl = [sb(f"tbl{i}", [64, 15, 64], F32) for i in range(2)]
        sbuf = [sb(f"sbf{i}", [64, 12, 64], F32) for i in range(2)]
        pl = [sb(f"pl{i}", [64, 12, 64], BF16) for i in range(2)]
        pc = [sb(f"pc{i}", [128, 2, 64], BF16) for i in range(2)]
        rinv = sb("rinv", [128, 64], F32)
        it = 0
        vloc = v_d[0:NLAT + NHALO_T + NHALO_B, :].rearrange("(r p) c -> p r c", p=64)
        vctx = v_d[NLAT + NHALO_T + NHALO_B:NK1, :].rearrange("(t p) c -> p t c", p=128)
        for h in range(16):
            hi = h % 2
            k.dma(kts[hi][:], kT_d[h], R=["kT_d"], W=[f"kts{hi}"])
            k.dma(vs[hi][:], vloc[:, :, h * 128:(h + 1) * 128], R=["v_d"], W=[f"vs{hi}"])
            k.dma(vsc[hi][:], vctx[:, :, h * 128:(h + 1) * 128], R=["v_d"], W=[f"vsc{hi}"])
            k.dma(tbl[hi][:], tbl_d[h], W=[f"tbl{hi}"])
            for j in range(16):
                ii = it % 2
                it += 1
                lrs = _lrs(j)
                nr = len(lrs)
                edge = j < 4 or j > 12
                q_ap = QT[:, h, j * 64:(j + 1) * 64]
                qkey = ("QT", h)
                pA, pB = b.ps[2 * ii], b.ps[2 * ii + 1]
                kA, kB = f"ps{2 * ii}", f"ps{2 * ii + 1}"
                pO, pS = b.ps[4 + ii], b.ps[6 + ii]
                kO, kS = f"ps{4 + ii}", f"ps{6 + ii}"
                for idx, lr in enumerate(lrs):
                    pp, kk, o = (pA, kA, idx) if idx < 8 else (pB, kB, idx - 8)
                    b.mm(pp[0:64, o * 64:(o + 1) * 64], kts[hi][:, _kcol(lr):_kcol(lr) + 64], q_ap, True, True, [f"kts{hi}", qkey], [kk])
                for c in range(2):
                    c0 = NLAT + NHALO_T + NHALO_B + c * 128
                    b.mm(pB[:, 256 + c * 64:256 + (c + 1) * 64], kts[hi][:, c0:c0 + 128], q_ap, True, True, [f"kts{hi}", qkey], [kB])
                a = lrs[0] - j + 3
                n1 = min(nr, 8)
                b.stt(sbuf[ii][:, 0:n1, :], pA[0:64, 0:n1 * 64].rearrange("p (r q) -> p r q", q=64), ATT_SCALE, tbl[hi][:, a:a + n1, :],
                      OP.mult, OP.add, [kA, f"tbl{hi}"], [f"sbf{ii}"])
                if nr > 8:
                    b.stt(sbuf[ii][:, 8:nr, :], pB[0:64, 0:(nr - 8) * 64].rearrange("p (r q) -> p r q", q=64), ATT_SCALE,
                          tbl[hi][:, a + 8:a + nr, :], OP.mult, OP.add, [kB, f"tbl{hi}"], [f"sbf{ii}"])
                if not edge:
                    b.act(pl[ii][:, 0:nr, :], sbuf[ii][:, 0:nr, :], AF.Exp, [f"sbf{ii}"], [f"pl{ii}"])
                else:
                    for idx in range(nr):
                        b.act(pl[ii][:, idx, :], sbuf[ii][:, idx, :], AF.Exp, [f"sbf{ii}", "rvb"], [f"pl{ii}"], bias=rvb[0:64, j, idx:idx + 1])
                b.act(pc[ii][:], pB[:, 256:384].rearrange("p (c q) -> p c q", q=64), AF.Exp, [kB], [f"pc{ii}"], scale=ATT_SCALE)
                nmm = nr + 2
                for idx, lr in enumerate(lrs):
                    vrow = _kcol(lr) // 64
                    b.mm(pO[:, 0:64], vs[hi][:, vrow, :], pl[ii][:, idx, :], idx == 0, False, [f"vs{hi}", f"pl{ii}"], [kO])
                    b.mm(pS[:, 0:64], b.ones_b[0:64, :], pl[ii][:, idx, :], idx == 0, False, ["ones_b", f"pl{ii}"], [kS])
                for c in range(2):
                    b.mm(pO[:, 0:64], vsc[hi][:, c, :], pc[ii][:, c, :], False, c == 1, [f"vsc{hi}", f"pc{ii}"], [kO])
                    b.mm(pS[:, 0:64], b.ones_b[:], pc[ii][:, c, :], False, c == 1, ["ones_b", f"pc{ii}"], [kS])
                b.recip(rinv[:], pS[:, 0:64], [kS], ["rinv"])
                b.tt("dve", q_ap, pO[:, 0:64], rinv[:], OP.mult, [kO, "rinv"], [qkey])
        k.barrier()

    with ExitStack() as es:
        sb = lambda n, s, d: es.enter_context(UT(nc, n, s, d))
        wst = [sb(f"wst{i}", [128, 16, 256], F32) for i in range(2)]
        wob = [sb(f"wob{i}", [128, 16, 256], BF16) for i in range(2)]
        it = 0
        for db in range(8):
            wi = db % 2
            b.load_w(wo[:, :, db * 256:(db + 1) * 256], wst[wi][:], f"wst{wi}", wob[wi][:], f"wob{wi}")
            for dcl in range(2):
                dc = 2 * db + dcl
                for tg, (c0, n, j) in enumerate(b.groups):
                    pb = it % 2
                    it += 1
                    for hc in range(16):
                        b.mm(b.ps[pb][:, :n], wob[wi][:, hc, dcl * 128:(dcl + 1) * 128], QT[:, hc, c0:c0 + n], hc == 0, hc == 15,
                             [f"wob{wi}", ("QT", hc)], [f"ps{pb}"])
                    b.stt(xT[:, dc, c0:c0 + n], b.ps[pb][:, :n], b.mG(0, j, dc), xT[:, dc, c0:c0 + n], OP.mult, OP.add,
                          [f"ps{pb}", "modT", xkey(dc, tg)], [xkey(dc, tg)])
        k.barrier()
    es_q.close()
    if not (b.debug_stop and "mix" in b.debug_stop):
        moe(b, xT, xkey, None)


NEDGE = 512
EDGE_TILES = ((0, 128), (128, 64), (768, 128), (896, 128), (1024, 64))


def build_fused(n_experts=NE, debug_stop=None):
    b = Builder(0, n_experts, debug_stop)
    nc, k = b.nc, b.k
    b.sfx = "_l0"
    es_all = ExitStack()
    sbA = lambda n, s, d: es_all.enter_context(UT(nc, n, s, d))
    b.setup_consts(es_all)
    rvb = sbA("rvb", [128, 16, 12], F32)
    gidx = sbA("gidx", [128, 6], mybir.dt.int32)
    k.dma(rvb[:], b.inp("rowbias", [128, 16, 12]), W=["rvb"])
    k.dma(gidx[:], b.inp("gidx", [128, 6], mybir.dt.int32), W=["gidx"])
    with ExitStack() as es_tmp:
        b.setup_vectors(es_all, es_tmp)
        k.barrier()
    xT = sbA("xT", [128, 16, NLAT + NCTX], F32)
    xkey = lambda kc, tg: ("xT", kc, tg)
    layer0_body(b, xT, xkey)

    NCHK = NEDGE // 16
    edge_t = nc.dram_tensor("edge_d", [NCHK, 128, 256], F32)
    gath_t = nc.dram_tensor("gath_d", [NCHK, 1024, 256], F32)
    edge_d = edge_t.ap().rearrange("c (r a) b -> (c r) (a b)", a=8)
    gath_v = gath_t.ap().rearrange("c (r a) b -> (c r) (a b)", a=8)
    gath_d = nc.dram_tensor("gath2_d", [8 * NEDGE, D], F32, kind="Internal").ap()
    with ExitStack() as es:
        ost = [es.enter_context(UT(nc, f"ost{i}", [128, D], F32)) for i in range(2)]
        ev = 0
        row = 0
        for ti, (t0, n) in enumerate(EDGE_TILES):
            tg = min(t0 // 512, 2)
            oi = ti % 2
            for m in range(4):
                pb = ev % 2
                for jj in range(4):
                    kc = 4 * m + jj
                    b.tr(b.ps[pb][0:n, jj * 128:(jj + 1) * 128], xT[:, kc, t0:t0 + n], b.ident[:], [xkey(kc, tg), "ident"], [f"ps{pb}"])
                b.cp("act" if ev % 2 == 0 else "dve", ost[oi][0:n, m * 512:(m + 1) * 512], b.ps[pb][0:n, :], [f"ps{pb}"], [f"ost{oi}"])
                ev += 1
            k.dma(edge_d[row:row + n, :], ost[oi][0:n, :], R=[f"ost{oi}"], W=["edge_d"])
            row += n
        k.barrier()
    if not (debug_stop and "nocc" in debug_stop):
        csem = nc.alloc_semaphore(name="cc_sem")
        for c in range(NCHK):
            nc.gpsimd.collective_compute("AllGather", OP.bypass, replica_groups=[list(range(8))],
                                         ins=[edge_t.ap()[c].opt()], outs=[gath_t.ap()[c].opt()]).then_inc(csem)
            nc.gpsimd.wait_ge(csem, c + 1)
    ccd = sbA("ccd", [128, 1], F32)
    k.op("pool", lambda: nc.gpsimd.memset(ccd[:], 0.0), W=["gath_v", "ccd"])
    k.barrier()
    for q8 in range(8):
        k.dma(gath_d[q8 * 512:(q8 + 1) * 512, :], gath_v[q8 * 512:(q8 + 1) * 512, :], R=["gath_v"], W=["gath_d"])
    k.barrier()

    b.set_layer(1)
    b.sfx = "_l1"
    with ExitStack() as es_tmp:
        b.setup_vectors(es_all, es_tmp)
        k.barrier()
    layer1_body(b, xT, xkey, rvb, gather=(gath_d, gidx))
    yout = nc.dram_tensor("yout", [NLAT, D], F32, kind="ExternalOutput").ap()
    store_tokens(b, xT, xkey, yout, NLAT)
    return b


def store_tokens(b, xT, xkey, out_d, ntok):
    nc, k = b.nc, b.k
    with ExitStack() as es:
        ost = [es.enter_context(UT(nc, f"ost{i}", [128, D], F32)) for i in range(2)]
        ev = 0
        nt = (ntok + 127) // 128
        for t in range(nt):
            n = min(128, ntok - t * 128)
            tg = min(t // 4, 2)
            oi = t % 2
            for m in range(4):
                pb = ev % 2
                for jj in range(4):
                    kc = 4 * m + jj
                    b.tr(b.ps[pb][0:n, jj * 128:(jj + 1) * 128], xT[:, kc, t * 128:t * 128 + n], b.ident[:],
                         [xkey(kc, tg), "ident"], [f"ps{pb}"])
                b.cp("act" if ev % 2 == 0 else "dve", ost[oi][0:n, m * 512:(m + 1) * 512], b.ps[pb][0:n, :], [f"ps{pb}"], [f"ost{oi}"])
                ev += 1
            k.dma(out_d[t * 128:t * 128 + n, :], ost[oi][0:n, :], R=[f"ost{oi}"], W=["out_d"])
    k.barrier()


def moe(b, xT, xkey, es_all):
    nc, k = b.nc, b.k
    NOWN = b.NOWN
    NT = (NOWN + 127) // 128
    rw_d = b.inp("router_w", [D, NE]).rearrange("(c p) e -> p c e", p=128)
    rb_d = b.inp("router_b", [1, NE])
    w1_d = b.inp("exp_w1", [b.n_experts, D, 2 * D])
    b1_d = b.inp("exp_b1", [NE, 2 * D])
    w2_d = b.inp("exp_w2", [b.n_experts, D, D])
    b2_d = b.inp("exp_b2", [NE, D])
    with ExitStack() as es:
        sb = lambda n, s, d: es.enter_context(UT(nc, n, s, d))
        hx = sb("hx", [128, 16, NOWN], BF16)
        gatesT = sb("gatesT", [NE, NOWN], F32)
        gsel = sb("gsel", [NE, NOWN], F32)
        b1g = sb("b1g", [128, NE * 16], F32)
        b1l = sb("b1l", [128, NE * 16], F32)
        with ExitStack() as es2:
            sb2 = lambda n, s, d: es2.enter_context(UT(nc, n, s, d))
            rw = sb2("rw", [128, 16, NE], F32)
            rb = sb2("rb", [1, NE], F32)
            h32 = [sb2(f"h32_{i}", [128, 512], F32) for i in range(2)]
            tm = {"sq": [sb2(f"sq{i}", [128, 512], F32) for i in range(2)], "rs": sb2("rs", [128, 512], F32),
                  "tmp": [sb2(f"tmp{i}", [128, 512], F32) for i in range(2)]}
            lg = sb2("lg", [128, NT, NE], F32)
            ex = sb2("ex", [128, NT, NE], F32)
            msk = sb2("msk", [128, NT, NE], F32)
            gts = sb2("gts", [128, NT, NE], F32)
            m8 = sb2("m8", [128, NT, 8], F32)
            nmx = sb2("nmx", [128, NT], F32)
            den = sb2("den", [128, NT], F32)
            b1t = sb2("b1t", [128, 256], F32)
            k.dma(rw[:], rw_d, W=["rw"])
            k.dma(rb[:], rb_d, W=["rb"])
            b1v = b1_d.rearrange("e (fc x) -> (e fc) x", x=256)
            for r in range(4):
                k.dma(b1t[:], b1v[r * 128:(r + 1) * 128, :], W=["b1t"])
                bv = b1t[:].rearrange("r (f two) -> r two f", two=2)
                for two, dst in ((0, b1g), (1, b1l)):
                    b.tr(b.ps[7][:, 0:128], bv[:, two, :], b.ident[:], ["b1t", "ident"], ["ps7"])
                    b.cp("dve", dst[:, r * 128:(r + 1) * 128], b.ps[7][:, 0:128], ["ps7"], ["b1"])
            lgp = b.ps[6][:, 0:NT * NE].rearrange("p (t e) -> p t e", e=NE)
            first = [True]
            n_mm = sum(16 * ((n + 127) // 128) for (_, n, _) in b.groups) + NT
            cnt_mm = [0]

            def rmm(out, lhsT, rhs, R):
                cnt_mm[0] += 1
                b.mm(out, lhsT, rhs, first[0], cnt_mm[0] == n_mm, R, ["ps6"], skip_group_check=True)
                first[0] = False

            for tg, (c0, n, j) in enumerate(b.groups):
                def post(kc, c0=c0, n=n):
                    i = kc % 2
                    for t0 in range(0, n, 128):
                        nt = min(128, n - t0)
                        tile = (c0 + t0) // 128
                        rmm(lgp[0:nt, tile, :], h32[i][:, t0:t0 + nt], rw[:, kc, :], [f"h32_{i}", "rw"])
                b.norm_mod(lambda kc: xT[:, kc, c0:c0 + n], lambda kc: xkey(kc, tg), n, 1, j,
                           lambda kc: (hx[:, kc, c0:c0 + n], ("hx", tg)), tm, b.ps[2], "ps2",
                           f32_fn=lambda kc: (h32[kc % 2][:, :n], f"h32_{kc % 2}"), post_fn=post)
            for t in range(NT):
                nt = min(128, NOWN - t * 128)
                rmm(lgp[0:nt, t, :], b.ones_f[0:1, 0:nt], rb[0:1, :], ["ones_f", "rb"])
            b.cp("dve", lg[:], lgp, ["ps6"], ["lg"])
            for t in range(NT):
                nt = min(128, NOWN - t * 128)
                k.op("dve", lambda: b.V.max(out=m8[0:nt, t, :], in_=lg[0:nt, t, :]), ["lg"], ["m8"])
                b.ts("dve", nmx[0:nt, t:t + 1], m8[0:nt, t, 0:1], -1.0, None, OP.mult, None, ["m8"], ["nmx"])
                b.ts("dve", msk[0:nt, t, :], lg[0:nt, t, :], m8[0:nt, t, 3:4], None, OP.is_ge, None, ["lg", "m8"], ["msk"])
                b.act(ex[0:nt, t, :], lg[0:nt, t, :], AF.Exp, ["lg", "nmx"], ["ex"], bias=nmx[0:nt, t:t + 1])
                k.op("dve", lambda: b.V.scalar_tensor_tensor(ex[0:nt, t, :], ex[0:nt, t, :], 1.0, msk[0:nt, t, :], OP.mult, OP.mult,
                                                             accum_out=den[0:nt, t:t + 1]), ["ex", "msk"], ["ex", "den"])
                b.recip(den[0:nt, t:t + 1], den[0:nt, t:t + 1], ["den"], ["den"])
                b.ts("dve", gts[0:nt, t, :], ex[0:nt, t, :], den[0:nt, t:t + 1], None, OP.mult, None, ["ex", "den"], ["gts"])
                b.tr(b.ps[7][0:NE, 0:nt], gts[0:nt, t, :], b.ident[0:nt, 0:nt], ["gts", "ident"], ["ps7"])
                b.cp("dve", gatesT[:, t * 128:t * 128 + nt], b.ps[7][0:NE, 0:nt], ["ps7"], ["gatesT"])
            k.barrier()

        with ExitStack() as es2:
            sb2 = lambda n, s, d: es2.enter_context(UT(nc, n, s, d))
            stg = [sb2(f"stg{i}", [128, 2048], F32) for i in range(3)]
            w1b = [sb2(f"w1b{i}", [128, 16, 2, 128], BF16) for i in range(2)]
            w2b = [sb2(f"w2b{i}", [128, 4, 512], BF16) for i in range(2)]
            actT = sb2("actT", [128, 4, NOWN], BF16)
            Gsb = sb2("Gsb", [128, NOWN], F32)
            tms = [[sb2(f"e{a}_{i}", [128, 512], F32) for a in range(3)] for i in range(2)]
            sctr = [0]
            w1ctr = [0]
            w2ctr = [0]
            ectr = [0]
            yctr = [0]

            def stage():
                i = sctr[0] % 3
                sctr[0] += 1
                return stg[i], f"stg{i}"

            for e in range(b.n_experts):
                b.ts("dve", gsel[:], gatesT[:], b.ident[0:NE, e:e + 1], None, OP.mult, None, ["gatesT", "ident"], ["gsel"])
                for tg, (c0, n, j) in enumerate(b.groups):
                    b.mm(b.ps[6][:, :n], b.ones_f[0:NE, :], gsel[:, c0:c0 + n], True, True, ["ones_f", "gsel"], ["ps6"])
                    b.cp("act", Gsb[:, c0:c0 + n], b.ps[6][:, :n], ["ps6"], [("Gsb", tg)])
                w1e = w1_d[e].rearrange("(c p) m -> p c m", p=128)
                w2e = w2_d[e].rearrange("(c p) m -> p c m", p=128)
                for qt in range(4):
                    for fcl in range(4):
                        fc = 4 * qt + fcl
                        wi = w1ctr[0] % 2
                        w1ctr[0] += 1
                        for hh in range(2):
                            st, skey = stage()
                            sv = st[:].rearrange("p (c m) -> p c m", c=8)
                            k.dma(sv, w1e[:, hh * 8:(hh + 1) * 8, fc * 256:(fc + 1) * 256], W=[skey])
                            svd = sv.rearrange("p c (f two) -> p c two f", two=2)
                            b.cp("act", w1b[wi][:, hh * 8:(hh + 1) * 8, 0, :], svd[:, :, 0, :], [skey], [f"w1b{wi}"])
                            b.cp("pool", w1b[wi][:, hh * 8:(hh + 1) * 8, 1, :], svd[:, :, 1, :], [skey], [f"w1b{wi}"])
                        for tg, (c0, n, j) in enumerate(b.groups):
                            ei = ectr[0] % 2
                            ectr[0] += 1
                            pg, pl = b.ps[2 * ei], b.ps[2 * ei + 1]
                            kg, kl = f"ps{2 * ei}", f"ps{2 * ei + 1}"
                            for two, (pp, kk) in enumerate(((pg, kg), (pl, kl))):
                                for kc in range(16):
                                    b.mm(pp[:, :n], w1b[wi][:, kc, two, :], hx[:, kc, c0:c0 + n], kc == 0, kc == 15,
                                         [f"w1b{wi}", ("hx", tg)], [kk])
                            t1, t2, t3 = tms[ei]
                            n1, n2, n3 = (f"e{a}_{ei}" for a in range(3))
                            col = e * 16 + fc
                            b.ts("dve", t1[:, :n], pg[:, :n], b1g[:, col:col + 1], SWIGLU_LIMIT, OP.add, OP.min, [kg, "b1"], [n1])
                            b.act(t2[:, :n], t1[:, :n], AF.Sigmoid, [n1], [n2], scale=SWIGLU_ALPHA)
                            b.act(t3[:, :n], pl[:, :n], AF.Identity, [kl, "b1"], [n3], bias=b1l[:, col:col + 1])
                            b.ts("pool", t3[:, :n], t3[:, :n], SWIGLU_LIMIT, -SWIGLU_LIMIT, OP.min, OP.max, [n3], [n3])
                            b.tt("pool", t2[:, :n], t1[:, :n], t2[:, :n], OP.mult, [n1, n2], [n2])
                            b.tt("pool", t2[:, :n], t2[:, :n], Gsb[:, c0:c0 + n], OP.mult, [n2, ("Gsb", tg)], [n2])
                            b.stt(actT[:, fcl, c0:c0 + n], t3[:, :n], 1.0, t2[:, :n], OP.add, OP.mult, [n2, n3], [("actT", fcl, tg)])
                    for db in range(4):
                        wi = w2ctr[0] % 2
                        w2ctr[0] += 1
                        st, skey = stage()
                        sv = st[:].rearrange("p (c m) -> p c m", c=4)
                        k.dma(sv, w2e[:, 4 * qt:4 * qt + 4, db * 512:(db + 1) * 512], W=[skey])
                        b.cp("act" if db % 2 == 0 else "pool", w2b[wi][:], sv, [skey], [f"w2b{wi}"])
                        for dcl in range(4):
                            dc = 4 * db + dcl
                            for tg, (c0, n, j) in enumerate(b.groups):
                                pb = 4 + yctr[0] % 2
                                yctr[0] += 1
                                for fcl in range(4):
                                    b.mm(b.ps[pb][:, :n], w2b[wi][:, fcl, dcl * 128:(dcl + 1) * 128], actT[:, fcl, c0:c0 + n],
                                         fcl == 0, fcl == 3, [f"w2b{wi}", ("actT", fcl, tg)], [f"ps{pb}"])
                                b.stt(xT[:, dc, c0:c0 + n], b.ps[pb][:, :n], b.mG(1, j, dc), xT[:, dc, c0:c0 + n], OP.mult, OP.add,
                                      [f"ps{pb}", "modT", xkey(dc, tg)], [xkey(dc, tg)])
            k.barrier()
        with ExitStack() as es2:
            b2 = es2.enter_context(UT(nc, "b2", [NE, D], F32))
            k.dma(b2[:], b2_d, W=["b2"])
            it = 0
            for dc in range(16):
                for tg, (c0, n, j) in enumerate(b.groups):
                    pb = 4 + it % 2
                    it += 1
                    b.mm(b.ps[pb][:, :n], b2[:, dc * 128:(dc + 1) * 128], gatesT[:, c0:c0 + n], True, True, ["b2", "gatesT"], [f"ps{pb}"])
                    b.stt(xT[:, dc, c0:c0 + n], b.ps[pb][:, :n], b.mG(1, j, dc), xT[:, dc, c0:c0 + n], OP.mult, OP.add,
                          [f"ps{pb}", "modT", xkey(dc, tg)], [xkey(dc, tg)])
            k.barrier()


def _consts():
    rot = np.zeros((128, 128), np.float32)
    for d in range(128):
        i = d % 64
        if i < 32:
            rot[d, d + 32] = -1.0
        else:
            rot[d, d - 32] = 1.0
    return {
        "c_ident": np.eye(128, dtype=np.float32),
        "c_ones": np.ones((128, 128), np.float32),
        "c_rotT": np.ascontiguousarray(rot.T).astype(ml_dtypes.bfloat16),
        "c_eps": np.full((128, 1), EPS, np.float32),
    }


def _rope_tables():
    m = 32
    inv_freq = (np.float32(10000.0) ** (-np.arange(m, dtype=np.float32) / np.float32(m))).astype(np.float32)
    t = np.arange(S)
    pos = [(t // 64).astype(np.float32), (t % 64).astype(np.float32)]
    cos = np.zeros((128, S), np.float32)
    sin = np.zeros((128, S), np.float32)
    for d in range(128):
        half, f = d // 64, d % 32
        ang = (pos[half] * inv_freq[f]).astype(np.float32)
        cos[d] = np.cos(ang)
        sin[d] = np.sin(ang)
    return cos, sin


_CACHE = {}


def _get_prog(name, fn):
    if name not in _CACHE:
        _CACHE[name] = fn()
    return _CACHE[name]


def _moe_inputs(inp, i):
    return {"router_w": inp["router_w"][i], "router_b": inp["router_b"][i][None, :], "exp_w1": inp["exp_w1"][i],
            "exp_b1": inp["exp_b1"][i], "exp_w2": inp["exp_w2"][i], "exp_b2": inp["exp_b2"][i]}


def layer0_inputs(inp, x, ctx):
    consts = _consts()
    cos, sin = _rope_tables()
    maps = []
    for core in range(8):
        bi, q = core // 4, core % 4
        perm = np.concatenate([np.arange(q * NLAT, (q + 1) * NLAT), np.arange(0, q * NLAT), np.arange((q + 1) * NLAT, S)])
        cperm = np.concatenate([np.arange(q * NCTX, (q + 1) * NCTX), np.arange(0, q * NCTX), np.arange((q + 1) * NCTX, LCTX)])
        m = dict(consts)
        m["xall"] = np.ascontiguousarray(x[bi][perm])
        m["ctxb"] = np.ascontiguousarray(ctx[bi][cperm])
        m["cosT"] = np.ascontiguousarray(cos[:, perm])
        m["sinT"] = np.ascontiguousarray(sin[:, perm])
        m["cvec"] = np.stack([inp["c"][bi], inp["c_ctx"]]).astype(np.float32)
        m["norm_g"] = np.stack([inp["norm_mix_g"][0], inp["norm_ffn_g"][0]])
        m["qk_gain"] = np.stack([inp["a_q_gain"][0], inp["a_k_gain"][0]])
        m["ada_w"] = inp["ada_w"][0]
        m["ada_b"] = inp["ada_b"][0]
        m["w_qkv"] = inp["a_w_qkv"][0]
        m["w_o"] = inp["a_w_o"][0]
        m.update(_moe_inputs(inp, 0))
        maps.append(m)
    return maps


def _bias_table(rel_bias):
    col = np.arange(64)
    cst = np.clip(col - 8, 0, 48)
    cmask = (col[None, :] >= cst[:, None]) & (col[None, :] < cst[:, None] + 16)
    dcidx = np.clip(col[None, :] - col[:, None], -15, 15) + 15
    g = rel_bias[:, :, dcidx]
    g = np.where(cmask[None, None], g, np.float32(NEG)).astype(np.float32)
    return np.ascontiguousarray(g.transpose(0, 3, 1, 2))


def _row_bias(q):
    R0 = 16 * q
    rb = np.full((128, 16, 12), NEG, np.float32)
    for j in range(16):
        r = R0 + j
        r0 = min(max(r - 4, 0), 56)
        for idx, lr in enumerate(_lrs(j)):
            gr = R0 - 4 + lr
            if 0 <= gr <= 63 and r0 <= gr < r0 + 8:
                rb[:, j, idx] = 0.0
    return rb


def layer1_inputs(inp, x1, ctx1):
    consts = _consts()
    tblh = _bias_table(inp["b_rel_bias"][0])
    maps = []
    for core in range(8):
        bi, q = core // 4, core % 4
        m = dict(consts)
        if x1 is not None:
            m["xown"] = np.ascontiguousarray(x1[bi, q * NLAT:(q + 1) * NLAT])
            oth = np.zeros((NHALO_T + NHALO_B + LCTX, D), np.float32)
            lo = q * NLAT - NHALO_T
            if lo >= 0:
                oth[0:NHALO_T] = x1[bi, lo:lo + NHALO_T]
            hi = (q + 1) * NLAT
            if hi + NHALO_B <= S:
                oth[NHALO_T:NHALO_T + NHALO_B] = x1[bi, hi:hi + NHALO_B]
            oth[NHALO_T + NHALO_B:] = ctx1[bi]
            m["xoth"] = oth
        m["rowbias"] = _row_bias(q)
        m["bias_tbl"] = tblh
        m["cvec"] = np.stack([inp["c"][bi], inp["c_ctx"]]).astype(np.float32)
        m["norm_g"] = np.stack([inp["norm_mix_g"][1], inp["norm_ffn_g"][1]])
        m["qk_gain"] = np.stack([inp["b_q_gain"][0], inp["b_k_gain"][0]])
        m["ada_w"] = inp["ada_w"][1]
        m["ada_b"] = inp["ada_b"][1]
        m["w_qkv"] = inp["b_w_qkv"][0]
        m["w_o"] = inp["b_w_o"][0]
        m.update(_moe_inputs(inp, 1))
        maps.append(m)
    return maps


def _gather_idx(core):
    bi, q = core // 4, core % 4
    up = core - 1 if q > 0 else core
    dn = core + 1 if q < 3 else core
    def grow(rank, er):
        er = np.asarray(er)
        return (er // 16) * 128 + rank * 16 + (er % 16)

    top = grow(up, 192 + np.arange(256))
    bot = grow(dn, np.arange(192))
    cx = np.concatenate([grow(4 * bi + cq, 448 + np.arange(64)) for cq in range(4)])
    allr = np.concatenate([top, bot, cx])
    tiles = [(0, 128), (128, 128), (256, 128), (384, 64), (448, 128), (576, 128)]
    g = np.zeros((128, 6), np.int32)
    for t, (r0, n) in enumerate(tiles):
        g[:n, t] = allr[r0:r0 + n]
    return g


def fused_inputs(inp):
    l0 = layer0_inputs(inp, inp["x"], inp["ctx"])
    l1 = layer1_inputs(inp, None, None)
    maps = []
    for core in range(8):
        m = {}
        for kk, v in l0[core].items():
            m[kk + "_l0" if kk in Builder.LAYER_INPUTS else kk] = v
        for kk, v in l1[core].items():
            if kk in Builder.LAYER_INPUTS:
                m[kk + "_l1"] = v
            elif kk in ("rowbias", "bias_tbl"):
                m[kk] = v
        m["gidx"] = _gather_idx(core)
        maps.append(m)
    return maps


def _run(bld, maps):
    maps = [{kk: np.ascontiguousarray(v) for kk, v in m.items() if kk in bld.din} for m in maps]
    return run_bass_kernel_spmd(bld.nc, maps, core_ids=list(range(8))).results


def kernel(**inputs):
    inp = {kk: np.asarray(v) for kk, v in inputs.items()}
    bld = _get_prog("fused", build_fused)
    r = _run(bld, fused_inputs(inp))
    out = np.zeros((2, S, D), np.float32)
    for core in range(8):
        bi, q = core // 4, core % 4
        out[bi, q * NLAT:(q + 1) * NLAT] = r[core]["yout"]
    return out


def kernel_unfused(**inputs):
    inp = {kk: np.asarray(v) for kk, v in inputs.items()}
    b0 = _get_prog("l0", build_layer0)
    r0 = _run(b0, layer0_inputs(inp, inp["x"], inp["ctx"]))
    x1 = np.zeros((2, S, D), np.float32)
    ctx1 = np.zeros((2, LCTX, D), np.float32)
    for core in range(8):
        bi, q = core // 4, core % 4
        o = r0[core]["x1"]
        x1[bi, q * NLAT:(q + 1) * NLAT] = o[:NLAT]
        ctx1[bi, q * NCTX:(q + 1) * NCTX] = o[NLAT:]
    b1 = _get_prog("l1", build_layer1)
    r1 = _run(b1, layer1_inputs(inp, x1, ctx1))
    out = np.zeros((2, S, D), np.float32)
    for core in range(8):
        bi, q = core // 4, core % 4
        out[bi, q * NLAT:(q + 1) * NLAT] = r1[core]["yout"]
    return out
```
